# Optimizing a Trainium2 kernel written in Bass

```python
import jax, jax.numpy as jnp
from jax import lax
import numpy as np

D_MODEL = 1024
BATCH = 4
SEQ = 8192
DEPTH = 2

CTX_LEN = 256
GRID_W = 64
N_DIR = 2
N_EVEN = (DEPTH + 1) // 2
N_ODD = DEPTH // 2
D_FF = 2816
MIX_W = 1024
EPS = 1e-6

RWKV_HEADS = 8
RWKV_HD = 64
RWKV_W = RWKV_HEADS * RWKV_HD
DECAY_LORA = 64
AAA_LORA = 64
GATE_LORA = 128
RWKV_GN_EPS = 64e-5
RWKV_SHIFT_W = 3 * RWKV_W + DECAY_LORA + AAA_LORA

MLSTM_HEADS = 4
MLSTM_HD = 128
MLSTM_W = MLSTM_HEADS * MLSTM_HD
MLSTM_CHUNK = 128

HGRN_HEADS = 4
HGRN_FD = 128
HGRN_ID = 128
HGRN_W = HGRN_HEADS * HGRN_FD
HGRN_CHUNK = 64

LRU_W = 512
LRU_BLOCKS = 8
LRU_BD = LRU_W // LRU_BLOCKS
CONV_W = 4
CONV_PAD_L = 2
LRU_C = 8.0

EVEN_SPLITS = (RWKV_W, RWKV_W, RWKV_W, N_DIR * DECAY_LORA, N_DIR * AAA_LORA, GATE_LORA,
               MLSTM_W, MLSTM_W, MLSTM_W, MLSTM_W, N_DIR * MLSTM_HEADS, N_DIR * MLSTM_HEADS)
ODD_SPLITS = (HGRN_W, N_DIR * HGRN_W, HGRN_W, HGRN_W, LRU_W, LRU_W)
EVEN_IN = sum(EVEN_SPLITS)
ODD_IN = sum(ODD_SPLITS)

kernel_name = 'hybrid_bidir_rwkv7_mlstm_hgrn2_rglru_dit'


def split_cols(t, sizes):
    idx = np.cumsum(sizes)[:-1].tolist()
    return jnp.split(t, idx, axis=-1)


def rmsnorm(x, g):
    xf = x.astype(jnp.float32)
    y = xf * lax.rsqrt(jnp.mean(xf * xf, axis=-1, keepdims=True) + EPS)
    return (y * g.astype(jnp.float32)).astype(x.dtype)


def frames_shared(t):
    return jnp.stack([t, jnp.flip(t, axis=1)])


def frames_dir(t):
    return jnp.stack([t[0], jnp.flip(t[1], axis=1)])


def merge_frames(o):
    return o[0] + jnp.flip(o[1], axis=1)


def shift_prev(t):
    return jnp.pad(t, ((0, 0), (0, 0), (1, 0), (0, 0)))[:, :, :-1]


def chunk_major(t, T):
    d, b, l, h = t.shape[:4]
    t = t.reshape((d, b, l // T, T, h) + t.shape[4:])
    return jnp.moveaxis(t, (2, 4), (0, 3))


def unchunk(t):
    t = jnp.moveaxis(t, (0, 3), (2, 4))
    d, b, nc, T, h = t.shape[:5]
    return t.reshape((d, b, nc * T, h) + t.shape[5:])


def to_col_major(t, rows):
    b, l, f = t.shape
    return t.reshape(b, rows, GRID_W, f).transpose(0, 2, 1, 3).reshape(b, l, f)


def to_row_major(t, rows):
    b, l, f = t.shape
    return t.reshape(b, GRID_W, rows, f).transpose(0, 2, 1, 3).reshape(b, l, f)


def swiglu(t, w1, w3, w2):
    return (jax.nn.silu(t @ w1) * (t @ w3)) @ w2


def bidir(seq, p, cols_ctx, cols_lat, state0):
    y_ctx, state = seq(cols_ctx, state0, p)
    y_lat, _ = seq(cols_lat, state, p)
    return y_ctx, y_lat


def rwkv7_seq(cols, S0, p):
    r, k, v, wd, ad, gd = (t.astype(jnp.float32) for t in cols)
    B_, L_ = r.shape[:2]
    heads = lambda t: t.reshape(t.shape[:3] + (RWKV_HEADS, RWKV_HD))
    per_dir = lambda t, n: frames_dir(jnp.moveaxis(t.reshape(B_, L_, N_DIR, n), 2, 0))
    s = jnp.concatenate([frames_shared(r), frames_shared(k), frames_shared(v),
                         per_dir(wd, DECAY_LORA), per_dir(ad, AAA_LORA)], axis=-1)
    s = s + p['mu'][:, None, None, :] * (shift_prev(s) - s)
    r, k, v, wd, ad = split_cols(s, (RWKV_W, RWKV_W, RWKV_W, DECAY_LORA, AAA_LORA))
    w = -jax.nn.softplus(-(p['w0'][:, None, None, :]
                           + jnp.einsum('dblr,drc->dblc', jnp.tanh(wd), p['w2']))) - 0.5
    a = jax.nn.sigmoid(p['a0'][:, None, None, :] + jnp.einsum('dblr,drc->dblc', ad, p['a2']))
    kk = heads(k * p['k_k'][:, None, None, :])
    kk = kk / jnp.maximum(jnp.sqrt(jnp.sum(kk * kk, axis=-1, keepdims=True)), 1e-12)
    k = heads(k * (1.0 + (a - 1.0) * p['k_a'][:, None, None, :]))
    r, v, a = heads(r), heads(v), heads(a)
    decay = heads(jnp.exp(-jnp.exp(w)))
    xs = tuple(jnp.moveaxis(t, 2, 0) for t in (r, decay, k, v, kk, kk * a))

    def step(S, inp):
        r_t, w_t, k_t, v_t, kk_t, b_t = inp
        S = (S * w_t[..., None, :]
             - jnp.einsum('...ij,...j->...i', S, kk_t)[..., :, None] * b_t[..., None, :]
             + v_t[..., :, None] * k_t[..., None, :])
        return S, jnp.einsum('...ij,...j->...i', S, r_t)

    S, o = lax.scan(step, S0, xs)
    o = jnp.moveaxis(o, 0, 2)
    mean = jnp.mean(o, axis=-1, keepdims=True)
    var = jnp.mean(jnp.square(o - mean), axis=-1, keepdims=True)
    gn = ((o - mean) * lax.rsqrt(var + RWKV_GN_EPS)).reshape(N_DIR, B_, L_, RWKV_W)
    gn = gn * p['ln_w'][:, None, None, :] + p['ln_b'][:, None, None, :]
    bonus = (jnp.sum(r * k * p['r_k'][:, None, None], axis=-1, keepdims=True) * v).reshape(N_DIR, B_, L_, RWKV_W)
    g = jnp.einsum('blr,rc->blc', jax.nn.sigmoid(gd), p['g2'])
    return merge_frames(gn + bonus) * g, S


def mlstm_seq(cols, state0, p):
    q, k, v, o_pre, i_pre, f_pre = (t.astype(jnp.float32) for t in cols)
    B_, L_ = q.shape[:2]
    T = MLSTM_CHUNK
    heads = lambda t: t.reshape(B_, L_, MLSTM_HEADS, MLSTM_HD)
    gates = lambda t, bias: frames_dir(jnp.moveaxis(t.reshape(B_, L_, N_DIR, MLSTM_HEADS), 2, 0)
                                       + bias[:, None, None, :])
    qc = chunk_major(frames_shared(heads(q)) * MLSTM_HD ** -0.5, T)
    kc = chunk_major(frames_shared(heads(k)), T)
    vc = chunk_major(frames_shared(heads(v)), T)
    li = chunk_major(gates(i_pre, p['b_i'])[..., None], T)[..., 0]
    lf = chunk_major(jax.nn.log_sigmoid(gates(f_pre, p['b_f']))[..., None], T)[..., 0]
    mask = jnp.tril(jnp.ones((T, T), bool))

    def step(carry, inp):
        C0, n0, m0 = carry
        q_c, k_c, v_c, li_c, lf_c = inp
        b = jnp.cumsum(lf_c, axis=-1)
        logD = jnp.where(mask, b[..., :, None] - b[..., None, :] + li_c[..., None, :], -jnp.inf)
        m_prev = b + m0[..., None]
        m = jnp.maximum(m_prev, jnp.max(logD, axis=-1))
        s = jnp.einsum('...tk,...sk->...ts', q_c, k_c) * jnp.exp(logD - m[..., None])
        inter = jnp.exp(m_prev - m)
        num = jnp.einsum('...ts,...sv->...tv', s, v_c) + inter[..., None] * jnp.einsum('...vk,...tk->...tv', C0, q_c)
        den = jnp.sum(s, axis=-1) + inter * jnp.einsum('...k,...tk->...t', n0, q_c)
        h = num / jnp.maximum(jnp.abs(den), jnp.exp(-m))[..., None]
        bT = b[..., -1]
        gsum = bT[..., None] - b + li_c
        m_new = jnp.maximum(bT + m0, jnp.max(gsum, axis=-1))
        wts = jnp.exp(gsum - m_new[..., None])
        carry_decay = jnp.exp(bT + m0 - m_new)
        C = carry_decay[..., None, None] * C0 + jnp.einsum('...t,...tv,...tk->...vk', wts, v_c, k_c)
        n = carry_decay[..., None] * n0 + jnp.einsum('...t,...tk->...k', wts, k_c)
        return (C, n, m_new), h

    state, h = lax.scan(step, state0, (qc, kc, vc, li, lf))
    h = merge_frames(unchunk(h))
    y = rmsnorm(h, p['ng'].reshape(MLSTM_HEADS, MLSTM_HD)).reshape(B_, L_, MLSTM_W)
    return y * jax.nn.sigmoid(o_pre), state


def hgrn2_seq(cols, S0, p):
    q, f_pre, i, g = (t.astype(jnp.float32) for t in cols)
    B_, L_ = q.shape[:2]
    T = HGRN_CHUNK
    lb = p['lb']
    qc = chunk_major(frames_shared(jax.nn.silu(q).reshape(B_, L_, HGRN_HEADS, HGRN_FD)), T)
    vc = chunk_major(frames_shared(i.reshape(B_, L_, HGRN_HEADS, HGRN_ID)), T)
    f = lb + (1.0 - lb) * jax.nn.sigmoid(f_pre.reshape(B_, L_, N_DIR, HGRN_W))
    f = frames_dir(jnp.moveaxis(f, 2, 0)).reshape(N_DIR, B_, L_, HGRN_HEADS, HGRN_FD)
    kc = chunk_major(1.0 - f, T)
    lfc = chunk_major(jnp.log(f), T)
    mask = jnp.tril(jnp.ones((T, T), bool))[..., None]

    def step(S, inp):
        q_c, k_c, v_c, lf_c = inp
        Bc = jnp.cumsum(lf_c, axis=-2)
        rel = jnp.exp(jnp.where(mask, Bc[..., :, None, :] - Bc[..., None, :, :], -jnp.inf))
        A = jnp.einsum('...tf,...tsf,...sf->...ts', q_c, rel, k_c)
        o = A @ v_c + jnp.einsum('...tf,...fi->...ti', q_c * jnp.exp(Bc), S)
        BT = Bc[..., -1:, :]
        S = jnp.exp(BT)[..., 0, :, None] * S + jnp.einsum('...sf,...si->...fi', k_c * jnp.exp(BT - Bc), v_c)
        return S, o

    S, o = lax.scan(step, S0, (qc, kc, vc, lfc))
    o = merge_frames(unchunk(o))
    y = rmsnorm(o, p['ng'].reshape(HGRN_HEADS, HGRN_ID)).reshape(B_, L_, HGRN_W)
    return y * jax.nn.silu(g), S


def rglru_seq(cols, h0, p):
    xb, gb = (t.astype(jnp.float32) for t in cols)
    B_, L_ = xb.shape[:2]
    xp = jnp.pad(xb, ((0, 0), (CONV_PAD_L, CONV_W - 1 - CONV_PAD_L), (0, 0)))
    xconv = sum(xp[:, j:j + L_] * p['conv_w'][j] for j in range(CONV_W)) + p['conv_b']
    xc = frames_shared(xconv)
    xh = xc.reshape(N_DIR, B_, L_, LRU_BLOCKS, LRU_BD)
    blockdiag = lambda w, b: (jnp.einsum('dblhi,dhij->dblhj', xh, w).reshape(N_DIR, B_, L_, LRU_W)
                              + b[:, None, None, :])
    r = jax.nn.sigmoid(blockdiag(p['wa'], p['ba']))
    ig = jax.nn.sigmoid(blockdiag(p['wi'], p['bi']))
    log_a = -LRU_C * r * jax.nn.softplus(-p['lam'])[:, None, None, :]
    a = jnp.exp(log_a)
    u = jnp.sqrt(-jnp.expm1(2.0 * log_a)) * (ig * xc)
    u = u.at[:, :, 0].add(a[:, :, 0] * h0)

    def combine(e1, e2):
        a1, b1 = e1
        a2, b2 = e2
        return a1 * a2, a2 * b1 + b2

    _, h = lax.associative_scan(combine, (a, u), axis=2)
    return merge_frames(h) * jax.nn.gelu(gb), h[:, :, -1]


def setup_inputs(seed: int = 0) -> dict:
    key = jax.random.key(seed)
    ks = list(jax.random.split(key, 48))
    f32 = jnp.float32

    def nrm(shape, scale):
        return jax.random.normal(ks.pop(), shape, f32) * scale

    def uni(shape, lo, hi):
        return jax.random.uniform(ks.pop(), shape, f32, lo, hi)

    D = D_MODEL
    ramp = (jnp.arange(RWKV_W, dtype=f32) / (RWKV_W - 1)) ** 0.9
    a_base = uni((N_ODD, N_DIR, LRU_W), 0.9, 0.999) ** (1.0 / LRU_C)
    return {
        'x': nrm((BATCH, SEQ, D), 1.0),
        'c': nrm((BATCH, D), 1.0),
        'ctx': nrm((BATCH, CTX_LEN, D), 1.0),
        'c_ctx': nrm((D,), 1.0),
        'norm1_g': 1.0 + nrm((DEPTH, D), 0.05),
        'norm2_g': 1.0 + nrm((DEPTH, D), 0.05),
        'mod_w': nrm((DEPTH, D, 6 * D), D ** -0.5),
        'mod_b': nrm((DEPTH, 6 * D), 0.02),
        'ffn_w1': nrm((DEPTH, D, D_FF), D ** -0.5),
        'ffn_w3': nrm((DEPTH, D, D_FF), D ** -0.5),
        'ffn_w2': nrm((DEPTH, D_FF, D), D_FF ** -0.5),
        'final_g': 1.0 + nrm((D,), 0.05),
        'ev_in_w': nrm((N_EVEN, D, EVEN_IN), D ** -0.5),
        'ev_out_w': nrm((N_EVEN, MIX_W, D), MIX_W ** -0.5),
        'rwkv_mu': uni((N_EVEN, N_DIR, RWKV_SHIFT_W), 0.0, 1.0),
        'rwkv_w0': -6.0 + 5.0 * ramp + nrm((N_EVEN, N_DIR, RWKV_W), 0.1),
        'rwkv_w2': nrm((N_EVEN, N_DIR, DECAY_LORA, RWKV_W), 0.5 * DECAY_LORA ** -0.5),
        'rwkv_a0': nrm((N_EVEN, N_DIR, RWKV_W), 0.1),
        'rwkv_a2': nrm((N_EVEN, N_DIR, AAA_LORA, RWKV_W), AAA_LORA ** -0.5),
        'rwkv_kk': 0.85 + nrm((N_EVEN, N_DIR, RWKV_W), 0.05),
        'rwkv_ka': 1.0 + nrm((N_EVEN, N_DIR, RWKV_W), 0.05),
        'rwkv_rk': nrm((N_EVEN, N_DIR, RWKV_HEADS, RWKV_HD), 0.1),
        'rwkv_lnw': 1.0 + nrm((N_EVEN, N_DIR, RWKV_W), 0.05),
        'rwkv_lnb': nrm((N_EVEN, N_DIR, RWKV_W), 0.02),
        'rwkv_g2': nrm((N_EVEN, GATE_LORA, RWKV_W), GATE_LORA ** -0.5),
        'mlstm_bi': nrm((N_EVEN, N_DIR, MLSTM_HEADS), 0.1),
        'mlstm_bf': jnp.linspace(3.0, 6.0, MLSTM_HEADS, dtype=f32) + nrm((N_EVEN, N_DIR, MLSTM_HEADS), 0.1),
        'mlstm_ng': 1.0 + nrm((N_EVEN, MLSTM_W), 0.05),
        'od_in_w': nrm((N_ODD, D, ODD_IN), D ** -0.5),
        'od_out_w': nrm((N_ODD, MIX_W, D), MIX_W ** -0.5),
        'hgrn_lb': 1.0 + nrm((DEPTH, HGRN_W), 0.1),
        'hgrn_ng': 1.0 + nrm((N_ODD, HGRN_W), 0.05),
        'lru_conv_w': nrm((N_ODD, CONV_W, LRU_W), CONV_W ** -0.5),
        'lru_conv_b': nrm((N_ODD, LRU_W), 0.02),
        'lru_wa': nrm((N_ODD, N_DIR, LRU_BLOCKS, LRU_BD, LRU_BD), LRU_BD ** -0.5),
        'lru_ba': nrm((N_ODD, N_DIR, LRU_W), 0.1),
        'lru_wi': nrm((N_ODD, N_DIR, LRU_BLOCKS, LRU_BD, LRU_BD), LRU_BD ** -0.5),
        'lru_bi': nrm((N_ODD, N_DIR, LRU_W), 0.1),
        'lru_lam': jnp.log(a_base) - jnp.log1p(-a_base),
    }


def reference(x, c, ctx, c_ctx, norm1_g, norm2_g, mod_w, mod_b, ffn_w1, ffn_w3, ffn_w2, final_g,
              ev_in_w, ev_out_w, rwkv_mu, rwkv_w0, rwkv_w2, rwkv_a0, rwkv_a2, rwkv_kk, rwkv_ka,
              rwkv_rk, rwkv_lnw, rwkv_lnb, rwkv_g2, mlstm_bi, mlstm_bf, mlstm_ng,
              od_in_w, od_out_w, hgrn_lb, hgrn_ng, lru_conv_w, lru_conv_b, lru_wa, lru_ba,
              lru_wi, lru_bi, lru_lam):
    B_, L_, _ = x.shape
    rows = L_ // GRID_W
    f32 = jnp.float32
    sm = jax.nn.softmax(hgrn_lb.astype(f32), axis=0)
    lower_bounds = jnp.cumsum(sm, axis=0) - sm[0]
    h, hc = x, ctx
    for l in range(DEPTH):
        j = l // 2
        last = l == DEPTH - 1
        mod = jnp.split((jax.nn.silu(c) @ mod_w[l] + mod_b[l])[:, None, :], 6, axis=-1)
        mod_c = jnp.split(jax.nn.silu(c_ctx) @ mod_w[l] + mod_b[l], 6, axis=-1)
        u = rmsnorm(h, norm1_g[l]) * (1.0 + mod[1]) + mod[0]
        uc = rmsnorm(hc, norm1_g[l]) * (1.0 + mod_c[1]) + mod_c[0]
        if l % 2 == 0:
            cols = split_cols(u @ ev_in_w[j], EVEN_SPLITS)
            cols_c = split_cols(uc @ ev_in_w[j], EVEN_SPLITS)
            pa = {'mu': rwkv_mu[j], 'w0': rwkv_w0[j], 'w2': rwkv_w2[j], 'a0': rwkv_a0[j],
                  'a2': rwkv_a2[j], 'k_k': rwkv_kk[j], 'k_a': rwkv_ka[j], 'r_k': rwkv_rk[j],
                  'ln_w': rwkv_lnw[j], 'ln_b': rwkv_lnb[j], 'g2': rwkv_g2[j]}
            pb = {'b_i': mlstm_bi[j], 'b_f': mlstm_bf[j], 'ng': mlstm_ng[j]}
            s_a = jnp.zeros((N_DIR, B_, RWKV_HEADS, RWKV_HD, RWKV_HD), f32)
            s_b = (jnp.zeros((N_DIR, B_, MLSTM_HEADS, MLSTM_HD, MLSTM_HD), f32),
                   jnp.zeros((N_DIR, B_, MLSTM_HEADS, MLSTM_HD), f32),
                   jnp.zeros((N_DIR, B_, MLSTM_HEADS), f32))
            y1_c, y1_l = bidir(rwkv7_seq, pa, cols_c[:6], cols[:6], s_a)
            y2_c, y2_l = bidir(mlstm_seq, pb, cols_c[6:], cols[6:], s_b)
            mix = jnp.concatenate([y1_l, y2_l], axis=-1).astype(h.dtype) @ ev_out_w[j]
            out_w = ev_out_w[j]
        else:
            cols = split_cols(to_col_major(u, rows) @ od_in_w[j], ODD_SPLITS)
            cols_c = split_cols(uc @ od_in_w[j], ODD_SPLITS)
            pc = {'lb': lower_bounds[l], 'ng': hgrn_ng[j]}
            pd = {'conv_w': lru_conv_w[j], 'conv_b': lru_conv_b[j], 'wa': lru_wa[j], 'ba': lru_ba[j],
                  'wi': lru_wi[j], 'bi': lru_bi[j], 'lam': lru_lam[j]}
            s_c = jnp.zeros((N_DIR, B_, HGRN_HEADS, HGRN_FD, HGRN_ID), f32)
            s_d = jnp.zeros((N_DIR, B_, LRU_W), f32)
            y1_c, y1_l = bidir(hgrn2_seq, pc, cols_c[:4], cols[:4], s_c)
            y2_c, y2_l = bidir(rglru_seq, pd, cols_c[4:], cols[4:], s_d)
            mix = to_row_major(jnp.concatenate([y1_l, y2_l], axis=-1).astype(h.dtype), rows) @ od_out_w[j]
            out_w = od_out_w[j]
        h = h + mod[2] * mix
        v = rmsnorm(h, norm2_g[l]) * (1.0 + mod[4]) + mod[3]
        h = h + mod[5] * swiglu(v, ffn_w1[l], ffn_w3[l], ffn_w2[l])
        if not last:
            mix_c = jnp.concatenate([y1_c, y2_c], axis=-1).astype(hc.dtype) @ out_w
            hc = hc + mod_c[2] * mix_c
            vc = rmsnorm(hc, norm2_g[l]) * (1.0 + mod_c[4]) + mod_c[3]
            hc = hc + mod_c[5] * swiglu(vc, ffn_w1[l], ffn_w3[l], ffn_w2[l])
    return rmsnorm(h, final_g)
```

```python
import numpy as np
from contextlib import ExitStack
import concourse.bass as bass
import concourse.mybir as mybir
from concourse.bass_utils import run_bass_kernel_spmd

F32 = mybir.dt.float32
BF16 = mybir.dt.bfloat16
AF = mybir.ActivationFunctionType
ALU = mybir.AluOpType
AX = mybir.AxisListType

D = 1024
L = 8192
LC = 256
NPOS = L + LC
DFF = 2816
EPS = 1e-6
GRID_W = 64
ROWS = L // GRID_W


class Tk:
    __slots__ = ("w", "r")

    def __init__(self):
        self.w = None
        self.r = []


class View:
    __slots__ = ("ap", "k")

    def __init__(self, ap, k):
        self.ap = ap
        self.k = k


class Buf:
    def __init__(self, t):
        self.t = t
        self.k = Tk()

    def __getitem__(self, idx):
        return View(self.t[idx], self.k)

    def v(self, ap):
        return View(ap, self.k)


class Prog:
    EPOCH = 16000
    NDMA = 24

    def __init__(self, nc, stack):
        self.nc = nc
        self.stack = stack
        self.eng = {"pe": nc.tensor, "act": nc.scalar, "dve": nc.vector, "pool": nc.gpsimd, "sp": nc.sync}
        self.sem = {}
        self.seq = {}
        self.nsem = 0
        for e in self.eng:
            self._new_sem(e)
        self.known = {e: {} for e in self.eng}
        self.dslots = []
        for i in range(self.NDMA):
            s = stack.enter_context(nc.semaphore("dq%d" % i))
            self.dslots.append([s, 0])
        self.dnext = 0
        self.ninstr = 0
        self.dq = 0

    def _new_sem(self, e):
        self.nsem += 1
        self.sem[e] = self.stack.enter_context(self.nc.semaphore("s_%s_%d" % (e, self.nsem)))
        self.seq[e] = 0

    def _wait(self, e, ev):
        sem, val = ev
        k = self.known[e]
        if k.get(sem.name, 0) >= val:
            return
        self.eng[e].wait_ge(sem, val)
        k[sem.name] = val

    def _deps(self, e, reads, writes, same=True):
        evs = []
        for t in reads:
            if t.w is not None:
                evs.append(t.w)
        for t in writes:
            if t.w is not None:
                evs.append(t.w)
            evs.extend(t.r)
        mysem = self.sem[e].name
        for ev in evs:
            if (not same) and ev[0].name == mysem:
                continue
            self._wait(e, ev)

    def _mark(self, ev, reads, writes):
        for t in writes:
            t.w = ev
            t.r = []
        for t in reads:
            if t in writes:
                continue
            t.r.append(ev)
            if len(t.r) > 32:
                t.r = t.r[-32:]

    def op(self, e, fn, reads=(), writes=(), same=None):
        if same is None:
            same = e != "pe"
        if self.seq[e] >= self.EPOCH:
            self._new_sem(e)
        self._deps(e, reads, writes, same)
        ins = fn(self.eng[e])
        self.seq[e] += 1
        ins.then_inc(self.sem[e], 1)
        ev = (self.sem[e], self.seq[e])
        self._mark(ev, reads, writes)
        self.ninstr += 1
        return ev

    def dma(self, out, in_, reads=(), writes=(), q=None, **kw):
        if q is None:
            q = ("sp", "pool")[self.dq % 2]
            self.dq += 1
        slot = self.dslots[self.dnext]
        self.dnext = (self.dnext + 1) % self.NDMA
        if slot[1] > 0:
            self._wait(q, (slot[0], slot[1]))
        if slot[1] >= self.EPOCH:
            slot[0] = self.stack.enter_context(self.nc.semaphore("dqx%d" % self.ninstr))
            slot[1] = 0
        self._deps(q, reads, writes, True)
        ins = self.eng[q].dma_start(out=out, in_=in_, **kw)
        slot[1] += 16
        ins.then_inc(slot[0], 16)
        ev = (slot[0], slot[1])
        self._mark(ev, reads, writes)
        self.ninstr += 1
        return ev

    def barrier(self):
        evs = [(self.sem[e], self.seq[e]) for e in self.eng if self.seq[e] > 0]
        evs += [(s[0], s[1]) for s in self.dslots if s[1] > 0]
        for e in self.eng:
            for ev in evs:
                if ev[0].name == self.sem[e].name:
                    continue
                self._wait(e, ev)


class KB:
    def __init__(self, nc, st):
        self.nc = nc
        self.st = st
        self.P = Prog(nc, st)
        self.nb = 0

    def sb(self, shape, dt=F32, name=None):
        self.nb += 1
        return Buf(self.st.enter_context(self.nc.sbuf_tensor(name or ("b%d" % self.nb), list(shape), dt)))

    def ps(self, shape, dt=F32, name=None):
        self.nb += 1
        return Buf(self.st.enter_context(self.nc.psum_tensor(name or ("p%d" % self.nb), list(shape), dt)))

    @staticmethod
    def _s(x):
        return x.ap if isinstance(x, View) else x

    @staticmethod
    def _k(*xs):
        return [x.k for x in xs if isinstance(x, View)]

    def tt(self, o, a, b, op, e="dve"):
        self.P.op(e, lambda E: E.tensor_tensor(o.ap, a.ap, b.ap, op), reads=self._k(a, b), writes=[o.k])

    def ts(self, o, a, s1, op0, s2=None, op1=None, e="dve"):
        if s2 is None:
            self.P.op(e, lambda E: E.tensor_single_scalar(o.ap, a.ap, self._s(s1), op0), reads=self._k(a, s1), writes=[o.k])
        else:
            self.P.op(e, lambda E: E.tensor_scalar(o.ap, a.ap, self._s(s1), self._s(s2), op0, op1),
                      reads=self._k(a, s1, s2), writes=[o.k])

    def stt(self, o, a, s, b, op0, op1, e="dve"):
        self.P.op(e, lambda E: E.scalar_tensor_tensor(o.ap, a.ap, self._s(s), b.ap, op0, op1),
                  reads=self._k(a, s, b), writes=[o.k])

    def cp(self, o, a, e="dve"):
        if e == "act":
            self.P.op(e, lambda E: E.copy(o.ap, a.ap), reads=[a.k], writes=[o.k])
        else:
            self.P.op(e, lambda E: E.tensor_copy(o.ap, a.ap), reads=[a.k], writes=[o.k])

    def memset(self, o, val, e="dve"):
        self.P.op(e, lambda E: E.memset(o.ap, val), writes=[o.k])

    def act(self, o, a, func, bias=None, scale=None, accum=None):
        kw = {}
        if bias is not None:
            kw["bias"] = self._s(bias)
        if scale is not None:
            kw["scale"] = self._s(scale)
        w = [o.k]
        if accum is not None:
            kw["accum_out"] = accum.ap
            w.append(accum.k)
        self.P.op("act", lambda E: E.activation(out=o.ap, in_=a.ap, func=func, **kw),
                  reads=self._k(a, bias, scale), writes=w)

    def recip(self, o, a):
        self.P.op("dve", lambda E: E.reciprocal(o.ap, a.ap), reads=[a.k], writes=[o.k])

    def scan(self, o, d0, d1, init):
        self.P.op("dve", lambda E: E.tensor_tensor_scan(o.ap, d0.ap, d1.ap, self._s(init), ALU.mult, ALU.add),
                  reads=self._k(d0, d1, init), writes=[o.k])

    def mm(self, o, lhsT, rhs, start=True, stop=True):
        self.P.op("pe", lambda E: E.matmul(o.ap, lhsT=lhsT.ap, rhs=rhs.ap, start=start, stop=stop),
                  reads=[lhsT.k, rhs.k], writes=[o.k])

    def tr(self, o, a, ident):
        self.P.op("pe", lambda E: E.transpose(o.ap, a.ap, ident.ap), reads=[a.k, ident.k], writes=[o.k])

    def ld(self, o, dram_ap, **kw):
        self.P.dma(o.ap, dram_ap, writes=[o.k], **kw)

    def stq(self, dram_ap, a, **kw):
        self.P.dma(dram_ap, a.ap, reads=[a.k], **kw)


def pv_layout():
    names = []
    for g in range(4):
        for j in range(4):
            names.append(("lru_cw", j, g))
        names.append(("lru_cb", g))
        for d in range(2):
            names.append(("lru_ba", d, g))
            names.append(("lru_bi", d, g))
            names.append(("lru_lam", d, g))
    for h in range(4):
        names.append(("hg_lb0", h))
        names.append(("hg_lb1", h))
        names.append(("hg_ng", h))
        names.append(("ml_ng", h))
        for d in range(2):
            names.append(("ml_bi", d, h))
            names.append(("ml_bf", d, h))
    for d in range(2):
        for c in range(4):
            for nm in ("mu_r", "mu_k", "mu_v", "w0", "a0", "k_k", "k_a", "r_k", "ln_w", "ln_b"):
                names.append((nm, d, c))
        names.append(("mu_wd", d))
        names.append(("mu_ad", d))
    return {n: i for i, n in enumerate(names)}


PVL = pv_layout()
NPV = len(PVL)
TB = 1024


def blocks(d):
    bl = [(0, LC)] + [(LC + j * TB, LC + (j + 1) * TB) for j in range(L // TB)]
    if d == 0:
        return [(lo, hi, False) for (lo, hi) in bl]
    lat = bl[1:]
    return [(0, LC, True)] + [(lo, hi, True) for (lo, hi) in reversed(lat)]


def blocksL(d):
    bl = [(LC + j * 512, LC + (j + 1) * 512) for j in range(L // 512)]
    if d == 0:
        return [(0, LC, False)] + [(lo, hi, False) for (lo, hi) in bl]
    return [(0, LC, True)] + [(lo, hi, True) for (lo, hi) in reversed(bl)]


def rv(buf, n, rev, off=0):
    ap = buf.t[:, off:off + n]
    if rev:
        ap = ap[:, ::-1]
    return View(ap, buf.k)


def build(debug=False):
    nc = bass.Bass("TRN2", target_bir_lowering=False)

    def din(name, shape, dt=F32):
        return nc.dram_tensor(name, list(shape), dt, kind="ExternalInput").ap()

    def dscr(name, shape, dt=F32):
        return nc.dram_tensor(name, list(shape), dt, kind=("ExternalOutput" if (debug and name in ("YT", "H")) else "Internal")).ap()

    x_in = din("x", [L, D])
    ctx_in = din("ctx", [LC, D])
    cvec = din("cvec", [2, D])
    n1g = din("n1g", [2, D])
    n2g = din("n2g", [2, D])
    fing = din("fing", [D])
    modw = din("modw", [2, D, 6 * D])
    modb = din("modb", [2, 6 * D])
    w1 = din("w1", [2, D, DFF])
    w3 = din("w3", [2, D, DFF])
    w2 = din("w2", [2, DFF, D])
    inw = [din("inw0", [D, 4096]), din("inw1", [D, 3584])]
    outw = [din("outw0", [D, D]), din("outw1", [D, D])]
    pv = din("pv", [128, NPV])
    identd = din("identd", [128, 128])
    eyeflat = din("eyeflat", [128 * 128])
    lruw = din("lruw", [16, 128, 128])
    rw2 = din("rw2", [2, 64, 512])
    ra2 = din("ra2", [2, 64, 512])
    rg2 = din("rg2", [128, 512])
    bonesd = din("bonesd", [128, 128])
    masksd = din("masksd", [6, 128, 128])
    out_d = nc.dram_tensor("out", [L, D], F32, kind="ExternalOutput").ap()
    TBLD = [dscr("TBL0", [NPOS, 2560]), dscr("TBL1", [NPOS, 2560])]
    VSD = [dscr("VS0", [512, NPOS]), dscr("VS1", [512, NPOS])]
    BND = [dscr("BN0", [512, NPOS]), dscr("BN1", [512, NPOS])]
    ORW = [dscr("ORW0", [512, NPOS]), dscr("ORW1", [512, NPOS])]

    H = dscr("H", [L, D])
    HC = dscr("HC", [LC, D])
    PT = dscr("PT", [4096, NPOS])
    YT = dscr("YT", [D, NPOS], BF16)
    VT = dscr("VT", [D, NPOS], BF16)
    AT = dscr("AT", [DFF, NPOS], BF16)
    OT = [dscr("OT0", [512, NPOS]), dscr("OT1", [512, NPOS])]
    MODS = dscr("MODS", [2, 6 * D])

    with ExitStack() as st:
        K = KB(nc, st)
        P = K.P
        pvt = K.sb([128, NPV], name="pvt")
        K.ld(pvt[:, :], pv[:, :])
        idf = K.sb([128, 128], name="idf")
        K.ld(idf[:, :], identd[:, :])
        idb = K.sb([128, 128], BF16, name="idb")
        K.cp(idb[:, :], idf[:, :])
        onesf = K.sb([128, 128], name="onesf")
        K.memset(onesf[:, :], 1.0)
        onesb = K.sb([128, 128], BF16, name="onesb")
        K.memset(onesb[:, :], 1.0)
        WA1 = K.sb([128, 16384], BF16, name="WA1")
        WA2 = K.sb([128, 16384], BF16, name="WA2")
        SA = K.sb([128, 4096], F32, name="SA")
        G4 = [K.sb([128, D], name="g4_%d" % i) for i in range(6)]
        G2 = [K.sb([128, D], BF16, name="g2_%d" % i) for i in range(2)]
        T8 = K.sb([128, 8, 512], BF16, name="T8")
        T2 = [K.sb([128, 8, 128], BF16, name="t2_%d" % i) for i in range(2)]
        ATt = K.sb([128, 22, 128], BF16, name="ATt")
        ATt2 = K.sb([128, 22, 128], BF16, name="ATt2")
        T2X = [K.sb([128, 8, 128], BF16, name="t2x_%d" % i) for i in range(2)]
        STG = [K.sb([128, 512], name="stg%d" % i) for i in range(3)]
        ASTG = [K.sb([128, 512], BF16, name="astg%d" % i) for i in range(3)]
        MX = [K.sb([128, TB + 8], name="mx%d" % i) for i in range(9)]
        MXB = [K.sb([128, TB], BF16, name="mxb%d" % i) for i in range(2)]
        SST = K.sb([128, 128], name="SST")
        NST = K.sb([128, 1], name="NST")
        ssb = K.sb([128, 1], name="ssb")
        smalls = K.sb([128, 16], name="smalls")
        LW = K.sb([128, 16, 128], name="LW")
        pbank = [K.ps([128, 512], name="pb%d" % i) for i in range(7)]
        pst = K.ps([128, 8, 128], BF16, name="pst")

        def pcol(*name):
            i = PVL[tuple(name)]
            return pvt[:, i:i + 1]

        def hrows(layer, seg, t0, n, src_override=None):
            if seg == 0:
                src = ctx_in if layer == 0 else HC
                return src[t0:t0 + n, :]
            src = x_in if layer == 0 else H
            if layer % 2 == 0:
                return src[t0:t0 + n, :]
            c = t0 // ROWS
            return src.rearrange("(r c) d -> c r d", c=GRID_W)[c]

        def hrows_out(layer, seg, t0, n, final=False):
            if seg == 0:
                return HC[t0:t0 + n, :]
            dst = out_d if final else H
            if layer % 2 == 0:
                return dst[t0:t0 + n, :]
            c = t0 // ROWS
            return dst.rearrange("(r c) d -> c r d", c=GRID_W)[c]

        SEGS = [(0, 0, LC), (1, LC, L)]

        def winv(c0, c1):
            if c1 <= 2048:
                return View(WA1.t[:, :].rearrange("p (k n) -> p k n", k=8)[:, :, c0:c1], WA1.k)
            return View(WA2.t[:, :].rearrange("p (k n) -> p k n", k=8)[:, :, c0 - 2048:c1 - 2048], WA2.k)

        def norm_mod(hin, A, B, outb, junk):
            K.act(junk, hin, AF.Square, accum=ssb[:, :])
            K.act(ssb[:, :], ssb[:, :], AF.Sqrt, scale=1.0 / D, bias=EPS)
            K.recip(ssb[:, :], ssb[:, :])
            K.stt(junk, hin, ssb[:, 0:1], A, ALU.mult, ALU.mult)
            K.tt(outb, junk, B, ALU.add)

        def load_mod(buf, layer, seg, i, gsrc=None):
            s = 1 if seg == 0 else 0
            K.ld(buf[:, :], MODS[s, i * D:(i + 1) * D].partition_broadcast(128))
            if gsrc is not None:
                K.ld(G4[5][:, :], gsrc.partition_broadcast(128))
                K.stt(buf[:, :], buf[:, :], 1.0, G4[5][:, :], ALU.add, ALU.mult)

        for layer in range(2):
            last = layer == 1
            ctile = smalls
            P.dma(smalls.t[:, 0:16].rearrange("p (s k) -> p s k", s=2), cvec.rearrange("s (k p) -> p s k", p=128),
                  writes=[smalls.k], q="sp", allow_slow_non_contiguous=True)
            K.act(smalls[:, 0:16], smalls[:, 0:16], AF.Silu)
            clhs = G4
            for s in range(2):
                for k in range(8):
                    K.cp(G4[s * 2 + k // 4][:, (k % 4) * 128:(k % 4 + 1) * 128] if False else
                         View(G4[s].t[:, k * 128:(k + 1) * 128], G4[s].k),
                         View(smalls.t[:, s * 8 + k:s * 8 + k + 1].to_broadcast([128, 128]), smalls.k))
            mwv = SA.t[:, :].rearrange("p (k n) -> p k n", k=8)
            for g in range(12):
                K.P.dma(mwv, modw[layer].rearrange("(k p) n -> p k n", p=128)[:, :, g * 512:(g + 1) * 512], writes=[SA.k])
                K.P.dma(STG[0].t[:, :], modb[layer, g * 512:(g + 1) * 512].partition_broadcast(128), writes=[STG[0].k])
                for s in range(2):
                    pb_ = pbank[s]
                    for k in range(8):
                        K.mm(pb_[:, :], View(G4[s].t[:, k * 128:(k + 1) * 128], G4[s].k), SA.v(mwv[:, k, :]), start=(k == 0), stop=(k == 7))
                    K.tt(STG[1 + s][:, :], pb_[:, :], STG[0][:, :], ALU.add)
                    K.stq(MODS[s:s + 1, g * 512:(g + 1) * 512], STG[1 + s][0:1, :])
            P.barrier()

            ncols = 4096 if layer == 0 else 3584
            nch = ncols // 128
            for g in range(ncols // 512):
                stg = SA.t[:, :].rearrange("p (k n) -> p k n", k=8)
                K.P.dma(stg, inw[layer].rearrange("(k p) n -> p k n", p=128)[:, :, g * 512:(g + 1) * 512], writes=[SA.k])
                K.cp(winv(g * 512, (g + 1) * 512), SA.v(stg))
            ci = 0
            for (seg, poff, slen) in SEGS:
                load_mod(G4[1], layer, seg, 1, n1g[layer])
                load_mod(G4[2], layer, seg, 0)
                bs = min(512, slen)
                for b0 in range(0, slen, bs):
                    for t in range(bs // 128):
                        hb_ = (G4[0], G4[4])[t % 2]
                        ub_ = G2[t % 2]
                        K.ld(hb_[:, :], hrows(layer, seg, b0 + t * 128, 128))
                        norm_mod(hb_[:, :], G4[1][:, :], G4[2][:, :], ub_[:, :], G4[3][:, :])
                        for k in range(8):
                            K.tr(pst[:, k, :], ub_[:, k * 128:(k + 1) * 128], idb[:, :])
                        K.cp(T8[:, :, t * 128:(t + 1) * 128], pst[:, :, :], e="act")
                    for c in range(nch):
                        pb_ = pbank[c % 6]
                        wv = winv(c * 128, (c + 1) * 128)
                        for k in range(8):
                            K.mm(pb_[:, 0:bs], View(wv.ap[:, k, :], wv.k), T8[:, k, 0:bs], start=(k == 0), stop=(k == 7))
                        sg = STG[ci % 3]
                        ci += 1
                        K.cp(sg[:, 0:bs], pb_[:, 0:bs], e=("act" if c % 2 else "dve"))
                        K.stq(PT[c * 128:(c + 1) * 128, poff + b0:poff + b0 + bs], sg[:, 0:bs])
            P.barrier()

            WAf = WA1.t[:, :].bitcast(F32)
            WBf = WA2.t[:, :].bitcast(F32)
            RX = [Buf(WAf[:, i * 520:(i + 1) * 520]) for i in range(15)]
            _o = [0]

            def cw(n):
                b = Buf(WBf[:, _o[0]:_o[0] + n])
                _o[0] += n
                return b
            MSK = cw(768)
            MK4 = [cw(512), cw(512)]
            RS = cw(1024)
            TMA = [cw(640), cw(640)]
            STp = [cw(128) for _ in range(4)]
            CT = cw(128)
            NBb = cw(128)
            SSh = cw(128)
            Dt = cw(128)
            St = cw(128)
            VK = cw(256)
            KW = cw(128)
            JK = cw(128)
            Am = cw(128)
            ccol = cw(8)
            wcol = cw(8)
            ebT = cw(8)
            GTc = cw(8)
            Dt2 = [Dt, cw(128)]
            St2 = [St, cw(128)]
            VK2 = [VK, cw(256)]
            KW2 = [KW, cw(128)]
            JK2 = [JK, cw(128)]
            JD2 = [cw(128), cw(128)]
            Am2 = [Am, cw(128)]
            cc2 = [ccol, cw(8)]
            wc2 = [wcol, cw(8)]
            eb2 = [ebT, cw(8)]
            MSK3 = MSK.t[:, :].rearrange("p (m q) -> p m q", m=6)
            K.P.dma(MSK3, masksd.rearrange("m p q -> p m q"), writes=[MSK.k])

            def msk(i):
                return View(MSK3[:, i, :], MSK.k)
            for d_ in range(2):
                for j_ in range(4):
                    K.cp(MK4[d_][:, j_ * 128:(j_ + 1) * 128], msk((4 if j_ % 2 == 0 else 2) + d_))

            def rst_mask(T):
                K.memset(RS[:, 0:1024], 1.0)
                K.memset(View(RS.t[:, 0:1024].rearrange("p (c t) -> p c t", t=T)[:, :, 0:1], RS.k), 0.0)

            def red(o, a):
                K.P.op("dve", lambda E: E.reduce_sum(o.ap, a.ap, AX.X), reads=[a.k], writes=[o.k])

            def merge(h, rows0_y, gate_rows0, gate_func, ngcol, lo0, hi0):
                for it_, lo in enumerate(range(lo0, hi0, 512)):
                    n = min(512, hi0 - lo)
                    par_ = it_ % 2
                    BX = MX if par_ == 0 else RX
                    K.ld(BX[0][:, 0:n], OT[0][h * 128:(h + 1) * 128, lo:lo + n])
                    K.ld(BX[1][:, 0:n], OT[1][h * 128:(h + 1) * 128, lo:lo + n])
                    K.ld(BX[2][:, 0:n], PT[gate_rows0 + h * 128:gate_rows0 + (h + 1) * 128, lo:lo + n])
                    K.tt(BX[0][:, 0:n], BX[0][:, 0:n], BX[1][:, 0:n], ALU.add)
                    K.tt(BX[1][:, 0:n], BX[0][:, 0:n], BX[0][:, 0:n], ALU.mult)
                    K.mm(pbank[3 * par_][:, 0:n], onesf[:, :], BX[1][:, 0:n])
                    K.act(BX[1][:, 0:n], pbank[3 * par_][:, 0:n], AF.Sqrt, scale=1.0 / 128, bias=EPS)
                    K.recip(BX[1][:, 0:n], BX[1][:, 0:n])
                    K.stt(BX[0][:, 0:n], BX[0][:, 0:n], ngcol, BX[1][:, 0:n], ALU.mult, ALU.mult)
                    K.act(BX[2][:, 0:n], BX[2][:, 0:n], gate_func)
                    K.tt(MXB[par_][:, 0:n], BX[0][:, 0:n], BX[2][:, 0:n], ALU.mult)
                    K.stq(YT[rows0_y + h * 128:rows0_y + (h + 1) * 128, lo:lo + n], MXB[par_][:, 0:n])


            if layer == 0:
                RWs = Buf(LW.t[0:64, :, :].rearrange("p a o -> p (a o)").rearrange("p (x y z) -> p x y z", x=2, y=2))
                RWs.k = LW.k
                RG2s = Buf(G4[5].t[:, 0:512])
                RG2s.k = G4[5].k
                bones = Buf(G4[4].t[:, 0:128])
                bones.k = G4[4].k
                K.ld(bones[:, :], bonesd[:, :])
                for d in range(2):
                    K.ld(RWs[:, 0, d, :], rw2[d])
                    K.ld(RWs[:, 1, d, :], ra2[d])
                K.ld(RG2s[:, :], rg2[:, :])

                def shifted(dst, row0, nrows, d, lo, hi, mucol):
                    n = hi - lo
                    slo, shi = (0, LC) if lo < LC else (LC, NPOS)
                    xh = MX[5]
                    K.memset(xh[0:nrows, 0:n + 2], 0.0)
                    a0_ = max(lo - 1, slo)
                    a1_ = min(hi + 1, shi)
                    K.ld(View(xh.t[0:nrows, a0_ - (lo - 1):a1_ - (lo - 1)], xh.k), PT[row0:row0 + nrows, a0_:a1_])
                    cur = xh[0:nrows, 1:1 + n]
                    prev = xh[0:nrows, 0:n] if d == 0 else xh[0:nrows, 2:2 + n]
                    K.tt(dst[0:nrows, 0:n], prev, cur, ALU.subtract)
                    K.stt(dst[0:nrows, 0:n], dst[0:nrows, 0:n], mucol, cur, ALU.mult, ALU.add)


                rst_mask(64)
                PADS = [MX[0], MX[1], MX[2], MX[3], MX[4], MX[7], MX[8]]
                for pb_ in PADS:
                    K.memset(pb_[:, 0:1024], 0.0)

                def pad3(buf):
                    return buf.t[:, 0:1024].rearrange("p (c t) -> p c t", t=128)

                def padq(buf, q):
                    return View(pad3(buf)[:, q, :], buf.k)

                def fill_pad(buf, src, nck, mul_bc=None):
                    for hh in range(2):
                        pr = slice(hh * 64, hh * 64 + 64)
                        o = View(pad3(buf)[pr, 0:nck, hh * 64:hh * 64 + 64], buf.k)
                        s3 = View(src.t[pr, 0:nck * 64].rearrange("p (c t) -> p c t", t=64), src.k)
                        if mul_bc is None:
                            K.cp(o, s3, e=("act" if hh else "dve"))
                        else:
                            K.tt(o, s3, View(mul_bc.t[pr, 0:nck].unsqueeze(2).to_broadcast([64, nck, 64]), mul_bc.k), ALU.mult)

                def blocksR(d):
                    bl = [(LC + j * 512, LC + (j + 1) * 512) for j in range(L // 512)]
                    if d == 0:
                        return [(0, LC, False)] + [(lo, hi, False) for (lo, hi) in bl]
                    return [(0, LC, True)] + [(lo, hi, True) for (lo, hi) in reversed(bl)]

                PA, PR, PB, PK, PV, PBG, PKG = PADS
                cnt = 0
                for d in range(2):
                    for c in range(4):
                        K.memset(STp[c][:, :], 0.0)
                    for (lo, hi, rev) in blocksR(d):
                        n = hi - lo
                        nck = n // 64
                        shifted(RX[10], 1536 + 64 * d, 64, d, lo, hi, View(pvt.t[0:64, PVL[("mu_wd", d)]:PVL[("mu_wd", d)] + 1], pvt.k))
                        K.act(RX[10][0:64, 0:n], RX[10][0:64, 0:n], AF.Tanh)
                        shifted(RX[11], 1664 + 64 * d, 64, d, lo, hi, View(pvt.t[0:64, PVL[("mu_ad", d)]:PVL[("mu_ad", d)] + 1], pvt.k))
                        for c in range(4):
                            cs = slice(c * 128, (c + 1) * 128)
                            shifted(RX[0], c * 128, 128, d, lo, hi, pcol("mu_r", d, c))
                            shifted(RX[1], 512 + c * 128, 128, d, lo, hi, pcol("mu_k", d, c))
                            shifted(RX[2], 1024 + c * 128, 128, d, lo, hi, pcol("mu_v", d, c))
                            K.mm(pbank[0][:, 0:n], RWs[:, 0, d, cs], RX[10][0:64, 0:n])
                            K.mm(pbank[1][:, 0:n], RWs[:, 1, d, cs], RX[11][0:64, 0:n])
                            K.act(RX[3][:, 0:n], pbank[0][:, 0:n], AF.Sigmoid, bias=pcol("w0", d, c))
                            K.ts(RX[3][:, 0:n], RX[3][:, 0:n], -0.6065306597126334, ALU.mult)
                            K.act(RX[4][:, 0:n], pbank[1][:, 0:n], AF.Sigmoid, bias=pcol("a0", d, c))
                            K.ts(RX[5][:, 0:n], RX[1][:, 0:n], pcol("k_k", d, c), ALU.mult)
                            K.tt(RX[9][:, 0:n], RX[5][:, 0:n], RX[5][:, 0:n], ALU.mult)
                            K.mm(pbank[2][:, 0:n], bones[:, :], RX[9][:, 0:n])
                            K.act(RX[9][:, 0:n], pbank[2][:, 0:n], AF.Sqrt)
                            K.ts(RX[9][:, 0:n], RX[9][:, 0:n], 1e-12, ALU.max)
                            K.recip(RX[9][:, 0:n], RX[9][:, 0:n])
                            K.tt(RX[5][:, 0:n], RX[5][:, 0:n], RX[9][:, 0:n], ALU.mult)
                            K.ts(RX[9][:, 0:n], RX[4][:, 0:n], -1.0, ALU.add, pcol("k_a", d, c), ALU.mult)
                            K.stt(RX[1][:, 0:n], RX[9][:, 0:n], 1.0, RX[1][:, 0:n], ALU.add, ALU.mult)
                            K.tt(RX[4][:, 0:n], RX[4][:, 0:n], RX[5][:, 0:n], ALU.mult)
                            K.stt(RX[9][:, 0:n], RX[0][:, 0:n], pcol("r_k", d, c), RX[1][:, 0:n], ALU.mult, ALU.mult)
                            K.mm(pbank[3][:, 0:n], bones[:, :], RX[9][:, 0:n])
                            K.tt(RX[9][:, 0:n], pbank[3][:, 0:n], RX[2][:, 0:n], ALU.mult)
                            K.stq(BND[d][cs, lo:hi], RX[9][:, 0:n])
                            K.scan(rv(RX[6], n, rev), RS[:, 0:n], rv(RX[3], n, rev), 0.0)
                            K.act(RX[7][:, 0:n], RX[6][:, 0:n], AF.Exp)
                            K.tt(RX[0][:, 0:n], RX[0][:, 0:n], RX[7][:, 0:n], ALU.mult)
                            K.tt(RX[9][:, 0:n], RX[6][:, 0:n], RX[3][:, 0:n], ALU.subtract)
                            K.act(RX[9][:, 0:n], RX[9][:, 0:n], AF.Exp)
                            K.stt(RX[8][:, 0:n], RX[5][:, 0:n], -1.0, RX[9][:, 0:n], ALU.mult, ALU.mult)
                            K.act(RX[9][:, 0:n], RX[6][:, 0:n], AF.Exp, scale=-1.0)
                            K.tt(RX[4][:, 0:n], RX[4][:, 0:n], RX[9][:, 0:n], ALU.mult)
                            K.tt(RX[1][:, 0:n], RX[1][:, 0:n], RX[9][:, 0:n], ALU.mult)
                            lastoff = 0 if rev else 63
                            K.cp(GTc[:, 0:nck], View(RX[7].t[:, 0:n].rearrange("p (c t) -> p c t", t=64)[:, :, lastoff], RX[7].k))
                            fill_pad(PA, RX[8], nck)
                            fill_pad(PR, RX[0], nck)
                            fill_pad(PB, RX[4], nck)
                            fill_pad(PK, RX[1], nck)
                            fill_pad(PV, RX[2], nck)
                            fill_pad(PBG, RX[4], nck, mul_bc=GTc)
                            fill_pad(PKG, RX[1], nck, mul_bc=GTc)
                            qs_ = list(range(nck))
                            if rev:
                                qs_.reverse()
                            ST = STp[c]
                            def mk_stages(q, p_):
                                sc = STG[p_]
                                g = G4[p_]
                                w = G4[2 + p_]
                                tm = TMA[p_]
                                b0, b1, b2 = pbank[3 * p_], pbank[3 * p_ + 1], pbank[3 * p_ + 2]
                                bs = pbank[6]
                                A_, R_, B_, K_, V_, BG_, KG_ = [padq(x, q) for x in PADS]
                                st = []

                                def s_scores():
                                    K.mm(b0[:, 0:128], B_, A_)
                                    K.mm(b0[:, 128:256], B_, R_)
                                    K.mm(b0[:, 256:384], K_, A_)
                                    K.mm(b0[:, 384:512], K_, R_)
                                    K.mm(b1[:, 0:128], A_, B_)
                                    K.mm(b1[:, 128:256], A_, idf[:, :])
                                    K.mm(b2[:, 0:128], V_, idf[:, :])
                                    K.mm(b2[:, 128:256], BG_, idf[:, :])
                                    K.mm(b2[:, 256:384], KG_, idf[:, :])
                                st.append(s_scores)

                                def s_evac1():
                                    K.tt(sc[:, :], b0[:, :], MK4[d][:, :], ALU.mult)
                                    K.tt(g[:, 0:128], b1[:, 0:128], msk(4 + (1 - d)), ALU.mult)
                                    K.cp(g[:, 128:256], b1[:, 128:256], e="act")
                                    K.cp(tm[:, 0:384], b2[:, 0:384], e="act")
                                st.append(s_evac1)

                                def s_av():
                                    K.mm(b1[:, 256:384], sc[:, 256:384], tm[:, 0:128])
                                    K.mm(b0[:, 0:128], sc[:, 0:128], g[:, 0:128])
                                    K.mm(b0[:, 128:256], g[:, 0:128], sc[:, 0:128])
                                st.append(s_av)

                                def s_evac2():
                                    K.cp(g[:, 256:384], b1[:, 256:384])
                                    K.cp(w[:, 0:256], b0[:, 0:256], e="act")
                                    K.tt(w[:, 256:384], idf[:, :], sc[:, 0:128], ALU.add)
                                st.append(s_evac2)
                                for k in range(1, 6):
                                    wi0 = ((k - 1) % 2) * 384
                                    wo0 = (k % 2) * 384
                                    if k < 5:
                                        def s_mm(wi0=wi0):
                                            K.mm(b0[:, 128:384], w[:, wi0:wi0 + 128], w[:, wi0 + 128:wi0 + 384])
                                            K.mm(b0[:, 0:128], w[:, wi0 + 128:wi0 + 256], w[:, wi0:wi0 + 128])

                                        def s_ev(wi0=wi0, wo0=wo0):
                                            K.cp(w[:, wo0:wo0 + 256], b0[:, 0:256], e="act")
                                            K.tt(w[:, wo0 + 256:wo0 + 384], w[:, wi0 + 256:wi0 + 384], b0[:, 256:384], ALU.add)
                                    else:
                                        def s_mm(wi0=wi0):
                                            K.mm(b0[:, 256:384], w[:, wi0:wi0 + 128], w[:, wi0 + 256:wi0 + 384])

                                        def s_ev(wi0=wi0, wo0=wo0):
                                            K.tt(g[:, 384:512], w[:, wi0 + 256:wi0 + 384], b0[:, 256:384], ALU.add)
                                    st.append(s_mm)
                                    st.append(s_ev)

                                def s_aw():
                                    K.mm(b1[:, 0:256], g[:, 384:512], g[:, 128:384])
                                st.append(s_aw)

                                def s_aw_ev():
                                    K.cp(tm[:, 384:640], b1[:, 0:256], e="act")
                                st.append(s_aw_ev)

                                def s_ghq():
                                    K.mm(b1[:, 384:512], tm[:, 384:512], tm[:, 128:256])
                                    K.mm(b2[:, 384:512], tm[:, 128:256], tm[:, 512:640], start=True, stop=False)
                                    K.mm(b2[:, 384:512], tm[:, 256:384], tm[:, 0:128], start=False, stop=True)
                                    K.mm(b1[:, 256:384], tm[:, 384:512], sc[:, 128:256])
                                st.append(s_ghq)

                                def s_ghq_ev():
                                    K.stt(g[:, 512:640], idf[:, :], GTc[:, q:q + 1], b1[:, 384:512], ALU.mult, ALU.add)
                                    K.cp(g[:, 640:768], b2[:, 384:512], e="act")
                                    K.tt(g[:, 768:896], b1[:, 256:384], R_, ALU.add)
                                st.append(s_ghq_ev)

                                def s_seq():
                                    o0 = p_ * 256
                                    K.mm(bs[:, o0:o0 + 128], ST[:, :], g[:, 768:896], start=True, stop=False)
                                    K.mm(bs[:, o0:o0 + 128], tm[:, 512:640], sc[:, 128:256], start=False, stop=False)
                                    K.mm(bs[:, o0:o0 + 128], tm[:, 0:128], sc[:, 384:512], start=False, stop=True)
                                    K.mm(bs[:, o0 + 128:o0 + 256], g[:, 512:640], ST[:, :])
                                    K.tt(ST[:, :], bs[:, o0 + 128:o0 + 256], g[:, 640:768], ALU.add)
                                    K.cp(g[:, 896:1024], bs[:, o0:o0 + 128], e="act")
                                    K.tt(RX[12][:, q * 64:(q + 1) * 64], g[:, 896:960], g[:, 960:1024], ALU.add)
                                return st, s_seq

                            for qi in range(0, nck, 2):
                                sa, seqa = mk_stages(qs_[qi], 0)
                                sb2, seqb = mk_stages(qs_[qi + 1], 1)
                                for fa, fb in zip(sa, sb2):
                                    fa()
                                    fb()
                                seqa()
                                seqb()
                            K.stq(ORW[d][cs, lo:hi], RX[12][:, 0:n])
                P.barrier()
                for c in range(4):
                    cs = slice(c * 128, (c + 1) * 128)
                    for it_, lo in enumerate(range(0, NPOS, 512)):
                        n = min(512, NPOS - lo)
                        par_ = it_ % 2
                        BX = MX if par_ == 0 else RX
                        for d in range(2):
                            K.ld(BX[0][:, 0:n], ORW[d][cs, lo:lo + n])
                            K.mm(pbank[3 * par_ + 0][:, 0:n], bones[:, :], BX[0][:, 0:n])
                            K.stt(BX[0][:, 0:n], pbank[3 * par_ + 0][:, 0:n], -1.0 / 64, BX[0][:, 0:n], ALU.mult, ALU.add)
                            K.tt(BX[1][:, 0:n], BX[0][:, 0:n], BX[0][:, 0:n], ALU.mult)
                            K.mm(pbank[3 * par_ + 1][:, 0:n], bones[:, :], BX[1][:, 0:n])
                            K.act(BX[1][:, 0:n], pbank[3 * par_ + 1][:, 0:n], AF.Sqrt, scale=1.0 / 64, bias=64e-5)
                            K.recip(BX[1][:, 0:n], BX[1][:, 0:n])
                            K.stt(BX[0][:, 0:n], BX[0][:, 0:n], pcol("ln_w", d, c), BX[1][:, 0:n], ALU.mult, ALU.mult)
                            K.ld(BX[2][:, 0:n], BND[d][cs, lo:lo + n])
                            K.stt(BX[3 + d][:, 0:n], BX[0][:, 0:n], pcol("ln_b", d, c), BX[2][:, 0:n], ALU.add, ALU.add)
                        K.tt(BX[3][:, 0:n], BX[3][:, 0:n], BX[4][:, 0:n], ALU.add)
                        K.ld(BX[5][:, 0:n], PT[1792:1920, lo:lo + n])
                        K.act(BX[5][:, 0:n], BX[5][:, 0:n], AF.Sigmoid)
                        K.mm(pbank[3 * par_ + 2][:, 0:n], RG2s[:, cs], BX[5][:, 0:n])
                        K.tt(MXB[par_][:, 0:n], BX[3][:, 0:n], pbank[3 * par_ + 2][:, 0:n], ALU.mult)
                        K.stq(YT[cs, lo:lo + n], MXB[par_][:, 0:n])
                P.barrier()
                rst_mask(128)
                for d in range(2):
                    for h in range(4):
                        K.memset(CT[:, :], 0.0)
                        K.memset(NBb[:, :], 0.0)
                        for (lo, hi, rev) in blocks(d):
                            n = hi - lo
                            K.ld(MX[0][:, 0:n], PT[3968 + 8 + d * 4 + h, lo:hi].partition_broadcast(128))
                            K.act(MX[0][:, 0:n], MX[0][:, 0:n], AF.Sigmoid, bias=pcol("ml_bf", d, h))
                            K.act(MX[0][:, 0:n], MX[0][:, 0:n], AF.Ln)
                            K.scan(rv(MX[6], n, rev), RS[:, 0:n], rv(MX[0], n, rev), 0.0)
                            K.ld(MX[1][:, 0:n], PT[3968 + d * 4 + h, lo:hi].partition_broadcast(128))
                            K.stt(MX[1][:, 0:n], MX[1][:, 0:n], pcol("ml_bi", d, h), MX[6][:, 0:n], ALU.add, ALU.subtract)
                            K.ld(MX[2][:, 0:n], PT[1920 + h * 128:1920 + (h + 1) * 128, lo:hi])
                            K.ts(MX[2][:, 0:n], MX[2][:, 0:n], 128.0 ** -0.5, ALU.mult)
                            K.act(MX[7][:, 0:n], MX[6][:, 0:n], AF.Exp)
                            K.tt(MX[7][:, 0:n], MX[7][:, 0:n], MX[2][:, 0:n], ALU.mult)
                            K.ld(MX[3][:, 0:n], PT[2432 + h * 128:2432 + (h + 1) * 128, lo:hi])
                            K.ld(MX[4][:, 0:n], PT[2944 + h * 128:2944 + (h + 1) * 128, lo:hi])
                            qs_ = list(range(n // 128))
                            if rev:
                                qs_.reverse()
                            def ml_stages(q, p_):
                                cs = slice(q * 128, (q + 1) * 128)
                                lastc = q * 128 if rev else q * 128 + 127
                                Dt_, St_, VK_, KW_, JK_, JD_ = Dt2[p_], St2[p_], VK2[p_], KW2[p_], JK2[p_], JD2[p_]
                                cc_, wc_, eb_ = cc2[p_], wc2[p_], eb2[p_]
                                a0, a1, a2 = pbank[3 * p_], pbank[3 * p_ + 1], pbank[3 * p_ + 2]

                                def ind():
                                    K.mm(a0[:, 0:128], MX[3][:, cs], MX[2][:, cs])
                                    K.mm(a1[:, 0:128], MX[4][:, cs], idf[:, :])
                                    K.mm(a1[:, 128:256], MX[3][:, cs], idf[:, :])
                                    K.tt(JK_[:, :], MX[1][:, cs], idf[:, :], ALU.mult)
                                    red(cc_[:, 0:1], JK_[:, :])
                                    K.act(Dt_[:, :], MX[6][:, cs], AF.Exp, bias=cc_[:, 0:1])
                                    K.tt(Dt_[:, :], Dt_[:, :], msk(d), ALU.mult)
                                    K.tt(St_[:, :], a0[:, 0:128], Dt_[:, :], ALU.mult)
                                    K.cp(VK_[:, :], a1[:, 0:256], e="act")
                                    K.act(wc_[:, 0:1], cc_[:, 0:1], AF.Exp, bias=MX[6][:, lastc:lastc + 1])
                                    K.act(eb_[:, 0:1], MX[6][:, lastc:lastc + 1], AF.Exp)
                                    K.ts(KW_[:, :], VK_[:, 128:256], wc_[:, 0:1], ALU.mult)

                                def seq():
                                    K.mm(a0[:, 128:256], VK_[:, 0:128], St_[:, :], start=True, stop=False)
                                    K.mm(a0[:, 128:256], CT[:, :], MX[7][:, cs], start=False, stop=True)
                                    K.mm(a1[:, 256:384], onesf[:, :], St_[:, :], start=True, stop=False)
                                    K.mm(a1[:, 256:384], NBb[:, :], MX[7][:, cs], start=False, stop=True)
                                    K.mm(a2[:, 0:128], KW_[:, :], VK_[:, 0:128])
                                    K.mm(a2[:, 128:256], KW_[:, :], onesf[:, :])
                                    K.stt(CT[:, :], CT[:, :], eb_[:, 0:1], a2[:, 0:128], ALU.mult, ALU.add)
                                    K.stt(NBb[:, :], NBb[:, :], eb_[:, 0:1], a2[:, 128:256], ALU.mult, ALU.add)
                                    K.act(JD_[:, :], a1[:, 256:384], AF.Abs)
                                    K.ts(JD_[:, :], JD_[:, :], 1.0, ALU.max)
                                    K.recip(JD_[:, :], JD_[:, :])
                                    K.tt(MX[8][:, cs], a0[:, 128:256], JD_[:, :], ALU.mult)
                                return ind, seq

                            for qi in range(0, len(qs_), 2):
                                ia, sa_ = ml_stages(qs_[qi], 0)
                                ib, sb2_ = ml_stages(qs_[qi + 1], 1)
                                ia()
                                ib()
                                sa_()
                                sb2_()
                            K.stq(OT[d][h * 128:(h + 1) * 128, lo:hi], MX[8][:, 0:n])
                P.barrier()
                for h in range(4):
                    merge(h, 512, 3456, AF.Sigmoid, pcol("ml_ng", h), 0, NPOS)
            else:
                rst_mask(64)
                for h in range(4):
                    K.tt(smalls[:, 0:1], pcol("hg_lb1", h), pcol("hg_lb0", h), ALU.subtract)
                    K.act(smalls[:, 0:1], smalls[:, 0:1], AF.Sigmoid)
                    K.ts(smalls[:, 1:2], smalls[:, 0:1], -1.0, ALU.mult, 1.0, ALU.add)
                    for d in range(2):
                        K.memset(SSh[:, :], 0.0)
                        for (lo, hi, rev) in blocks(d):
                            n = hi - lo
                            nsub = n // 64
                            K.ld(MX[0][:, 0:n], PT[512 + d * 512 + h * 128:512 + d * 512 + (h + 1) * 128, lo:hi])
                            K.act(MX[0][:, 0:n], MX[0][:, 0:n], AF.Sigmoid)
                            K.ts(MX[0][:, 0:n], MX[0][:, 0:n], smalls[:, 1:2], ALU.mult, smalls[:, 0:1], ALU.add)
                            K.act(MX[1][:, 0:n], MX[0][:, 0:n], AF.Ln)
                            K.scan(rv(MX[2], n, rev), RS[:, 0:n], rv(MX[1], n, rev), 0.0)
                            K.ts(MX[3][:, 0:n], MX[0][:, 0:n], -1.0, ALU.mult, 1.0, ALU.add)
                            K.act(MX[4][:, 0:n], MX[2][:, 0:n], AF.Exp)
                            K.ld(MX[5][:, 0:n], PT[h * 128:(h + 1) * 128, lo:hi])
                            K.act(MX[5][:, 0:n], MX[5][:, 0:n], AF.Silu)
                            K.tt(MX[5][:, 0:n], MX[5][:, 0:n], MX[4][:, 0:n], ALU.mult)
                            K.act(MX[6][:, 0:n], MX[2][:, 0:n], AF.Exp, scale=-1.0)
                            K.tt(MX[6][:, 0:n], MX[6][:, 0:n], MX[3][:, 0:n], ALU.mult)
                            lastoff = 0 if rev else 63
                            e3 = MX[4].t[:, 0:n].rearrange("p (c t) -> p c t", t=64)[:, :, lastoff:lastoff + 1].to_broadcast([128, nsub, 64])
                            K.tt(View(MX[7].t[:, 0:n].rearrange("p (c t) -> p c t", t=64), MX[7].k),
                                 View(MX[6].t[:, 0:n].rearrange("p (c t) -> p c t", t=64), MX[6].k),
                                 View(e3, MX[4].k), ALU.mult)
                            K.ld(MX[8][:, 0:n], PT[1536 + h * 128:1536 + (h + 1) * 128, lo:hi])
                            qs_ = list(range(n // 128))
                            if rev:
                                qs_.reverse()
                            def hg_stages(q, p_):
                                cs = slice(q * 128, (q + 1) * 128)
                                Am_, VK_, JK_ = Am2[p_], VK2[p_], JK2[p_]
                                a0, a1, a2 = pbank[3 * p_], pbank[3 * p_ + 1], pbank[3 * p_ + 2]

                                def ind():
                                    K.mm(a0[:, 0:128], MX[6][:, cs], MX[5][:, cs])
                                    K.mm(a1[:, 0:128], MX[8][:, cs], idf[:, :])
                                    K.mm(a1[:, 128:256], MX[7][:, cs], idf[:, :])
                                    K.tt(Am_[:, :], a0[:, 0:128], msk(2 + d), ALU.mult)
                                    K.cp(VK_[:, :], a1[:, 0:256], e="act")
                                    K.mm(a0[:, 128:256], VK_[:, 0:128], Am_[:, :])
                                    K.cp(JK_[:, :], a0[:, 128:256])

                                def seq():
                                    for sb_ in ((1, 0) if rev else (0, 1)):
                                        c0 = q * 128 + sb_ * 64
                                        pr = slice(sb_ * 64, sb_ * 64 + 64)
                                        K.mm(a2[:, sb_ * 64:sb_ * 64 + 64], SSh[:, :], MX[5][:, c0:c0 + 64])
                                        K.mm(a2[:, 128:256], VK_[pr, 128:256], VK_[pr, 0:128])
                                        K.stt(SSh[:, :], SSh[:, :], MX[4][:, c0 + lastoff:c0 + lastoff + 1], a2[:, 128:256], ALU.mult, ALU.add)
                                    K.tt(MX[1][:, cs], JK_[:, :], a2[:, 0:128], ALU.add)
                                return ind, seq

                            for qi in range(0, len(qs_), 2):
                                ia, sa_ = hg_stages(qs_[qi], 0)
                                ib, sb2_ = hg_stages(qs_[qi + 1], 1)
                                ia()
                                ib()
                                sa_()
                                sb2_()
                            K.stq(OT[d][h * 128:(h + 1) * 128, lo:hi], MX[1][:, 0:n])
                P.barrier()
                for h in range(4):
                    merge(h, 0, 2048, AF.Silu, pcol("hg_ng", h), LC, NPOS)
                P.barrier()
                K.ld(LW[:, :, :], lruw.rearrange("a i o -> i a o"))
                for g in range(4):
                    for d in range(2):
                        K.act(smalls[:, 2:3], pcol("lru_lam", d, g), AF.Exp, scale=-1.0)
                        K.act(smalls[:, 2:3], smalls[:, 2:3], AF.Ln, bias=1.0)
                        K.ts(smalls[:, 3:4], smalls[:, 2:3], -8.0, ALU.mult)
                        K.ts(smalls[:, 4:5], smalls[:, 2:3], -16.0, ALU.mult)
                        K.memset(NST[:, :], 0.0)
                        for bi_, (lo, hi, rev) in enumerate(blocksL(d)):
                            n = hi - lo
                            par_ = bi_ % 2
                            BX = MX if par_ == 0 else RX
                            slo, shi = (0, LC) if lo < LC else (LC, NPOS)
                            a0 = max(lo - 2, slo)
                            a1 = min(hi + 1, shi)
                            xh = BX[0]
                            K.memset(xh[:, 0:n + 3], 0.0)
                            K.ld(View(xh.t[:, a0 - (lo - 2):a1 - (lo - 2)], xh.k), PT[2560 + g * 128:2560 + (g + 1) * 128, a0:a1])
                            xc = BX[1]
                            K.ts(xc[:, 0:n], xh[:, 2:2 + n], pcol("lru_cw", 2, g), ALU.mult, pcol("lru_cb", g), ALU.add)
                            K.stt(xc[:, 0:n], xh[:, 0:n], pcol("lru_cw", 0, g), xc[:, 0:n], ALU.mult, ALU.add)
                            K.stt(xc[:, 0:n], xh[:, 1:1 + n], pcol("lru_cw", 1, g), xc[:, 0:n], ALU.mult, ALU.add)
                            K.stt(xc[:, 0:n], xh[:, 3:3 + n], pcol("lru_cw", 3, g), xc[:, 0:n], ALU.mult, ALU.add)
                            for c in range((n + 511) // 512):
                                cn = min(512, n - c * 512)
                                sl = slice(c * 512, c * 512 + cn)
                                K.mm(pbank[2 * par_][:, 0:cn], LW[:, (d * 2 + 0) * 4 + g, :], xc[:, sl])
                                K.mm(pbank[2 * par_ + 1][:, 0:cn], LW[:, (d * 2 + 1) * 4 + g, :], xc[:, sl])
                                K.act(BX[2][:, sl], pbank[2 * par_][:, 0:cn], AF.Sigmoid, bias=pcol("lru_ba", d, g))
                                K.act(BX[3][:, sl], pbank[2 * par_ + 1][:, 0:cn], AF.Sigmoid, bias=pcol("lru_bi", d, g))
                            K.act(BX[4][:, 0:n], BX[2][:, 0:n], AF.Exp, scale=smalls[:, 3:4])
                            K.act(BX[5][:, 0:n], BX[2][:, 0:n], AF.Exp, scale=smalls[:, 4:5])
                            K.act(BX[5][:, 0:n], BX[5][:, 0:n], AF.Sqrt, scale=-1.0, bias=1.0)
                            K.tt(BX[5][:, 0:n], BX[5][:, 0:n], BX[3][:, 0:n], ALU.mult)
                            K.tt(BX[5][:, 0:n], BX[5][:, 0:n], xc[:, 0:n], ALU.mult)
                            K.scan(rv(BX[6], n, rev), rv(BX[4], n, rev), rv(BX[5], n, rev), NST[:, 0:1])
                            lastc = 0 if rev else n - 1
                            K.cp(NST[:, 0:1], BX[6][:, lastc:lastc + 1], e="act")
                            if d == 0:
                                K.stq(OT[0][g * 128:(g + 1) * 128, lo:hi], BX[6][:, 0:n])
                            elif lo >= LC:
                                K.ld(BX[7][:, 0:n], OT[0][g * 128:(g + 1) * 128, lo:hi])
                                K.tt(BX[6][:, 0:n], BX[6][:, 0:n], BX[7][:, 0:n], ALU.add)
                                gb = BX[8]
                                K.ld(gb[:, 0:n], PT[3072 + g * 128:3072 + (g + 1) * 128, lo:hi])
                                K.tt(BX[7][:, 0:n], gb[:, 0:n], gb[:, 0:n], ALU.mult)
                                K.ts(BX[7][:, 0:n], BX[7][:, 0:n], 0.044715, ALU.mult, 1.0, ALU.add)
                                K.tt(BX[7][:, 0:n], BX[7][:, 0:n], gb[:, 0:n], ALU.mult)
                                K.act(BX[7][:, 0:n], BX[7][:, 0:n], AF.Sigmoid, scale=1.5957691216057308)
                                K.tt(BX[7][:, 0:n], BX[7][:, 0:n], gb[:, 0:n], ALU.mult)
                                K.tt(MXB[par_][:, 0:n], BX[6][:, 0:n], BX[7][:, 0:n], ALU.mult)
                                K.stq(YT[512 + g * 128:512 + (g + 1) * 128, lo:hi], MXB[par_][:, 0:n])
                        P.barrier()
            P.barrier()

            wob = WA1.t[:, 0:8 * D].rearrange("p (k n) -> p k n", k=8)
            for g in range(2):
                stg = SA.t[:, :].rearrange("p (k n) -> p k n", k=8)
                K.P.dma(stg, outw[layer].rearrange("(k p) n -> p k n", p=128)[:, :, g * 512:(g + 1) * 512], writes=[SA.k])
                K.cp(WA1.v(wob[:, :, g * 512:(g + 1) * 512]), SA.v(stg))
            for (seg, poff, slen) in SEGS:
                if last and seg == 0:
                    continue
                load_mod(G4[1], layer, seg, 4, n2g[layer])
                load_mod(G4[2], layer, seg, 3)
                load_mod(G4[4], layer, seg, 2)
                for t0 in range(0, slen, 128):
                    par = (t0 // 128) % 2
                    ytb = (T2[0], T2X[0])[par]
                    vtb = (T2[1], T2X[1])[par]
                    hb_ = (G4[0], G4[5])[par]
                    ub_ = G2[par]
                    K.ld(ytb[:, :, :], YT.rearrange("(k p) t -> p k t", p=128)[:, :, poff + t0:poff + t0 + 128])
                    K.ld(hb_[:, :], hrows(layer, seg, t0, 128))
                    for half in range(2):
                        pb_ = pbank[2 * par + half]
                        for k in range(8):
                            K.mm(pb_[:, :], ytb[:, k, :], WA1.v(wob[:, k, half * 512:(half + 1) * 512]), start=(k == 0), stop=(k == 7))
                        K.tt(G4[3][:, half * 512:(half + 1) * 512], pb_[:, :], G4[4][:, half * 512:(half + 1) * 512], ALU.mult)
                    K.tt(hb_[:, :], hb_[:, :], G4[3][:, :], ALU.add)
                    K.stq(hrows_out(layer, seg, t0, 128), hb_[:, :])
                    norm_mod(hb_[:, :], G4[1][:, :], G4[2][:, :], ub_[:, :], G4[3][:, :])
                    for k in range(8):
                        K.tr(pst[:, k, :], ub_[:, k * 128:(k + 1) * 128], idb[:, :])
                    K.cp(vtb[:, :, :], pst[:, :, :], e="act")
                    K.stq(VT.rearrange("(k p) t -> p k t", p=128)[:, :, poff + t0:poff + t0 + 128], vtb[:, :, :])
            P.barrier()

            HF = DFF // 2
            for hf in range(2):
                w1b = WA1.t[:, 0:8 * HF].rearrange("p (k n) -> p k n", k=8)
                w3b = WA2.t[:, 0:8 * HF].rearrange("p (k n) -> p k n", k=8)
                for (wsrc, wdst, wbuf) in ((w1, w1b, WA1), (w3, w3b, WA2)):
                    for g0 in range(0, HF, 512):
                        gn = min(512, HF - g0)
                        stg = SA.t[:, :].rearrange("p (k n) -> p k n", k=8)
                        K.P.dma(stg[:, :, 0:gn], wsrc[layer].rearrange("(k p) n -> p k n", p=128)[:, :, hf * HF + g0:hf * HF + g0 + gn], writes=[SA.k])
                        K.cp(wbuf.v(wdst[:, :, g0:g0 + gn]), SA.v(stg[:, :, 0:gn]))
                ci = 0
                for (seg, poff, slen) in SEGS:
                    if last and seg == 0:
                        continue
                    bs = min(512, slen)
                    for b0 in range(0, slen, bs):
                        K.ld(T8[:, :, 0:bs], VT.rearrange("(k p) t -> p k t", p=128)[:, :, poff + b0:poff + b0 + bs])
                        for fc in range(HF // 128):
                            p1 = pbank[(2 * fc) % 6]
                            p3 = pbank[(2 * fc + 1) % 6]
                            for k in range(8):
                                K.mm(p1[:, 0:bs], WA1.v(w1b[:, k, fc * 128:(fc + 1) * 128]), T8[:, k, 0:bs], start=(k == 0), stop=(k == 7))
                            for k in range(8):
                                K.mm(p3[:, 0:bs], WA2.v(w3b[:, k, fc * 128:(fc + 1) * 128]), T8[:, k, 0:bs], start=(k == 0), stop=(k == 7))
                            sl_ = STG[ci % 3]
                            K.act(sl_[:, 0:bs], p1[:, 0:bs], AF.Silu)
                            sg = ASTG[ci % 3]
                            ci += 1
                            K.tt(sg[:, 0:bs], sl_[:, 0:bs], p3[:, 0:bs], ALU.mult)
                            r0 = hf * HF + fc * 128
                            K.stq(AT[r0:r0 + 128, poff + b0:poff + b0 + bs], sg[:, 0:bs])
                P.barrier()

            def w2v(k, c0, c1):
                if k < 16:
                    return View(WA1.t[:, :].rearrange("p (k n) -> p k n", k=16)[:, k, c0:c1], WA1.k)
                return View(WA2.t[:, :].rearrange("p (k n) -> p k n", k=16)[:, k - 16, c0:c1], WA2.k)
            for kc in range(22):
                for g in range(1):
                    K.P.dma(SA.t[:, 0:D], w2[layer].rearrange("(k p) n -> p k n", p=128)[:, kc, :], writes=[SA.k])
                    K.cp(w2v(kc, 0, D), SA[:, 0:D])
            if last:
                K.ld(G4[2][:, :], fing.partition_broadcast(128))
            for (seg, poff, slen) in SEGS:
                if last and seg == 0:
                    continue
                load_mod(G4[4], layer, seg, 5)
                for t0 in range(0, slen, 128):
                    par = (t0 // 128) % 2
                    atb = (ATt, ATt2)[par]
                    hb_ = (G4[0], G4[5])[par]
                    tb_ = (G4[3], G4[1])[par]
                    K.ld(atb[:, :, :], AT.rearrange("(k p) t -> p k t", p=128)[:, :, poff + t0:poff + t0 + 128])
                    K.ld(hb_[:, :], hrows_out(layer, seg, t0, 128))
                    for half in range(2):
                        pb_ = pbank[2 * par + half]
                        for k in range(22):
                            K.mm(pb_[:, :], atb[:, k, :], w2v(k, half * 512, (half + 1) * 512), start=(k == 0), stop=(k == 21))
                        K.tt(tb_[:, half * 512:(half + 1) * 512], pb_[:, :], G4[4][:, half * 512:(half + 1) * 512], ALU.mult)
                    K.tt(hb_[:, :], hb_[:, :], tb_[:, :], ALU.add)
                    if last:
                        K.act(tb_[:, :], hb_[:, :], AF.Square, accum=ssb[:, :])
                        K.act(ssb[:, :], ssb[:, :], AF.Sqrt, scale=1.0 / D, bias=EPS)
                        K.recip(ssb[:, :], ssb[:, :])
                        K.stt(tb_[:, :], hb_[:, :], ssb[:, 0:1], G4[2][:, :], ALU.mult, ALU.mult)
                        K.stq(hrows_out(layer, seg, t0, 128, final=True), tb_[:, :])
                    else:
                        K.stq(hrows_out(layer, seg, t0, 128), hb_[:, :])
            P.barrier()
        print("instructions:", P.ninstr)
    return nc


def prep_inputs(inp):
    f = np.float32
    g = lambda k: np.asarray(inp[k], dtype=f)
    ev = g("ev_in_w")[0]
    splits = np.cumsum([0, 512, 512, 512, 128, 128, 128, 512, 512, 512, 512, 8, 8])
    inw0 = np.zeros((D, 4096), f)
    inw0[:, :3984] = ev
    pvv = np.zeros((128, NPV), f)

    def setc(name, vec):
        pvv[:, PVL[name]] = vec
    cw = g("lru_conv_w")[0]
    for gi in range(4):
        sl = slice(gi * 128, (gi + 1) * 128)
        for j in range(4):
            setc(("lru_cw", j, gi), cw[j, sl])
        setc(("lru_cb", gi), g("lru_conv_b")[0, sl])
        for d in range(2):
            setc(("lru_ba", d, gi), g("lru_ba")[0, d, sl])
            setc(("lru_bi", d, gi), g("lru_bi")[0, d, sl])
            setc(("lru_lam", d, gi), g("lru_lam")[0, d, sl])
    for h in range(4):
        sl = slice(h * 128, (h + 1) * 128)
        setc(("hg_lb0", h), g("hgrn_lb")[0, sl])
        setc(("hg_lb1", h), g("hgrn_lb")[1, sl])
        setc(("hg_ng", h), g("hgrn_ng")[0, sl])
        setc(("ml_ng", h), g("mlstm_ng")[0, sl])
        for d in range(2):
            setc(("ml_bi", d, h), np.full(128, g("mlstm_bi")[0, d, h], f))
            setc(("ml_bf", d, h), np.full(128, g("mlstm_bf")[0, d, h], f))
    mu = g("rwkv_mu")[0]
    for d in range(2):
        for c in range(4):
            sl = slice(c * 128, (c + 1) * 128)
            setc(("mu_r", d, c), mu[d, 0:512][sl])
            setc(("mu_k", d, c), mu[d, 512:1024][sl])
            setc(("mu_v", d, c), mu[d, 1024:1536][sl])
            setc(("w0", d, c), g("rwkv_w0")[0, d, sl])
            setc(("a0", d, c), g("rwkv_a0")[0, d, sl])
            setc(("k_k", d, c), g("rwkv_kk")[0, d, sl])
            setc(("k_a", d, c), g("rwkv_ka")[0, d, sl])
            setc(("r_k", d, c), g("rwkv_rk")[0, d].reshape(-1)[sl])
            setc(("ln_w", d, c), g("rwkv_lnw")[0, d, sl])
            setc(("ln_b", d, c), g("rwkv_lnb")[0, d, sl])
        pvv[0:64, PVL[("mu_wd", d)]] = mu[d, 1536:1600]
        pvv[0:64, PVL[("mu_ad", d)]] = mu[d, 1600:1664]
    bones_ = np.zeros((128, 128), f)
    bones_[0:64, 0:64] = 1.0
    bones_[64:128, 64:128] = 1.0
    ii = np.arange(128)
    sgrid, tgrid = np.meshgrid(ii, ii, indexing="ij")
    same = (sgrid // 64) == (tgrid // 64)
    masks_ = np.stack([sgrid <= tgrid, sgrid >= tgrid, same & (sgrid <= tgrid), same & (sgrid >= tgrid),
                       same & (sgrid < tgrid), same & (sgrid > tgrid)]).astype(f)
    lruw = np.zeros((2, 2, 4, 128, 128), f)
    for d in range(2):
        for wi_, nm in enumerate(("lru_wa", "lru_wi")):
            w = g(nm)[0, d]
            for gi in range(4):
                for bb in range(2):
                    lruw[d, wi_, gi, bb * 64:(bb + 1) * 64, bb * 64:(bb + 1) * 64] = w[gi * 2 + bb]
    shared = {
        "n1g": g("norm1_g"), "n2g": g("norm2_g"), "fing": g("final_g"),
        "modw": g("mod_w"), "modb": g("mod_b"), "w1": g("ffn_w1"), "w3": g("ffn_w3"), "w2": g("ffn_w2"),
        "inw0": inw0, "inw1": np.ascontiguousarray(g("od_in_w")[0]),
        "outw0": np.ascontiguousarray(g("ev_out_w")[0]), "outw1": np.ascontiguousarray(g("od_out_w")[0]),
        "pv": pvv, "identd": np.eye(128, dtype=f), "eyeflat": np.eye(128, dtype=f).reshape(-1),
        "lruw": lruw.reshape(16, 128, 128),
        "rw2": np.ascontiguousarray(g("rwkv_w2")[0]), "ra2": np.ascontiguousarray(g("rwkv_a2")[0]),
        "rg2": np.ascontiguousarray(g("rwkv_g2")[0]), "bonesd": bones_, "masksd": masks_,
    }
    maps = []
    for core in range(8):
        b = core % 4
        m = dict(shared)
        m["x"] = np.ascontiguousarray(g("x")[b])
        m["ctx"] = np.ascontiguousarray(g("ctx")[b])
        m["cvec"] = np.stack([g("c")[b], g("c_ctx")])
        maps.append(m)
    return maps


_NC = None


def kernel(**inputs):
    global _NC
    if _NC is None:
        _NC = build()
    maps = prep_inputs(inputs)
    res = run_bass_kernel_spmd(_NC, maps, core_ids=list(range(8)))
    return np.stack([res.results[b]["out"] for b in range(4)]).astype(np.float32)
```

```python
import numpy as np
from contextlib import ExitStack
import concourse.bass as bass
import concourse.mybir as mybir
from concourse.bass_utils import run_bass_kernel_spmd

F32 = mybir.dt.float32
BF16 = mybir.dt.bfloat16
AF = mybir.ActivationFunctionType
ALU = mybir.AluOpType
AX = mybir.AxisListType

D = 1024
L = 8192
LC = 256
NPOS = L + LC
DFF = 2816
EPS = 1e-6
GRID_W = 64
ROWS = L // GRID_W


class Tk:
    __slots__ = ("w", "r")

    def __init__(self):
        self.w = None
        self.r = []


class View:
    __slots__ = ("ap", "k")

    def __init__(self, ap, k):
        self.ap = ap
        self.k = k


class Buf:
    def __init__(self, t):
        self.t = t
        self.k = Tk()

    def __getitem__(self, idx):
        return View(self.t[idx], self.k)

    def v(self, ap):
        return View(ap, self.k)


class Prog:
    EPOCH = 16000
    NDMA = 24

    def __init__(self, nc, stack):
        self.nc = nc
        self.stack = stack
        self.eng = {"pe": nc.tensor, "act": nc.scalar, "dve": nc.vector, "pool": nc.gpsimd, "sp": nc.sync}
        self.sem = {}
        self.seq = {}
        self.nsem = 0
        for e in self.eng:
            self._new_sem(e)
        self.known = {e: {} for e in self.eng}
        self.dslots = []
        for i in range(self.NDMA):
            s = stack.enter_context(nc.semaphore("dq%d" % i))
            self.dslots.append([s, 0])
        self.dnext = 0
        self.ninstr = 0
        self.dq = 0

    def _new_sem(self, e):
        self.nsem += 1
        self.sem[e] = self.stack.enter_context(self.nc.semaphore("s_%s_%d" % (e, self.nsem)))
        self.seq[e] = 0

    def _wait(self, e, ev):
        sem, val = ev
        k = self.known[e]
        if k.get(sem.name, 0) >= val:
            return
        self.eng[e].wait_ge(sem, val)
        k[sem.name] = val

    def _deps(self, e, reads, writes, same=True):
        evs = []
        for t in reads:
            if t.w is not None:
                evs.append(t.w)
        for t in writes:
            if t.w is not None:
                evs.append(t.w)
            evs.extend(t.r)
        mysem = self.sem[e].name
        for ev in evs:
            if (not same) and ev[0].name == mysem:
                continue
            self._wait(e, ev)

    def _mark(self, ev, reads, writes):
        for t in writes:
            t.w = ev
            t.r = []
        for t in reads:
            if t in writes:
                continue
            t.r.append(ev)
            if len(t.r) > 32:
                t.r = t.r[-32:]

    def op(self, e, fn, reads=(), writes=(), same=None):
        if same is None:
            same = e != "pe"
        if self.seq[e] >= self.EPOCH:
            self._new_sem(e)
        self._deps(e, reads, writes, same)
        ins = fn(self.eng[e])
        self.seq[e] += 1
        ins.then_inc(self.sem[e], 1)
        ev = (self.sem[e], self.seq[e])
        self._mark(ev, reads, writes)
        self.ninstr += 1
        return ev

    def dma(self, out, in_, reads=(), writes=(), q=None, **kw):
        if q is None:
            q = ("sp", "pool")[self.dq % 2]
            self.dq += 1
        slot = self.dslots[self.dnext]
        self.dnext = (self.dnext + 1) % self.NDMA
        if slot[1] > 0:
            self._wait(q, (slot[0], slot[1]))
        if slot[1] >= self.EPOCH:
            slot[0] = self.stack.enter_context(self.nc.semaphore("dqx%d" % self.ninstr))
            slot[1] = 0
        self._deps(q, reads, writes, True)
        ins = self.eng[q].dma_start(out=out, in_=in_, **kw)
        slot[1] += 16
        ins.then_inc(slot[0], 16)
        ev = (slot[0], slot[1])
        self._mark(ev, reads, writes)
        self.ninstr += 1
        return ev

    def barrier(self):
        evs = [(self.sem[e], self.seq[e]) for e in self.eng if self.seq[e] > 0]
        evs += [(s[0], s[1]) for s in self.dslots if s[1] > 0]
        for e in self.eng:
            for ev in evs:
                if ev[0].name == self.sem[e].name:
                    continue
                self._wait(e, ev)


class KB:
    def __init__(self, nc, st):
        self.nc = nc
        self.st = st
        self.P = Prog(nc, st)
        self.nb = 0

    def sb(self, shape, dt=F32, name=None):
        self.nb += 1
        return Buf(self.st.enter_context(self.nc.sbuf_tensor(name or ("b%d" % self.nb), list(shape), dt)))

    def ps(self, shape, dt=F32, name=None):
        self.nb += 1
        return Buf(self.st.enter_context(self.nc.psum_tensor(name or ("p%d" % self.nb), list(shape), dt)))

    @staticmethod
    def _s(x):
        return x.ap if isinstance(x, View) else x

    @staticmethod
    def _k(*xs):
        return [x.k for x in xs if isinstance(x, View)]

    def tt(self, o, a, b, op, e="dve"):
        self.P.op(e, lambda E: E.tensor_tensor(o.ap, a.ap, b.ap, op), reads=self._k(a, b), writes=[o.k])

    def ts(self, o, a, s1, op0, s2=None, op1=None, e="dve"):
        if s2 is None:
            self.P.op(e, lambda E: E.tensor_single_scalar(o.ap, a.ap, self._s(s1), op0), reads=self._k(a, s1), writes=[o.k])
        else:
            self.P.op(e, lambda E: E.tensor_scalar(o.ap, a.ap, self._s(s1), self._s(s2), op0, op1),
                      reads=self._k(a, s1, s2), writes=[o.k])

    def stt(self, o, a, s, b, op0, op1, e="dve"):
        self.P.op(e, lambda E: E.scalar_tensor_tensor(o.ap, a.ap, self._s(s), b.ap, op0, op1),
                  reads=self._k(a, s, b), writes=[o.k])

    def cp(self, o, a, e="dve"):
        if e == "act":
            self.P.op(e, lambda E: E.copy(o.ap, a.ap), reads=[a.k], writes=[o.k])
        else:
            self.P.op(e, lambda E: E.tensor_copy(o.ap, a.ap), reads=[a.k], writes=[o.k])

    def memset(self, o, val, e="dve"):
        self.P.op(e, lambda E: E.memset(o.ap, val), writes=[o.k])

    def act(self, o, a, func, bias=None, scale=None, accum=None):
        kw = {}
        if bias is not None:
            kw["bias"] = self._s(bias)
        if scale is not None:
            kw["scale"] = self._s(scale)
        w = [o.k]
        if accum is not None:
            kw["accum_out"] = accum.ap
            w.append(accum.k)
        self.P.op("act", lambda E: E.activation(out=o.ap, in_=a.ap, func=func, **kw),
                  reads=self._k(a, bias, scale), writes=w)

    def recip(self, o, a):
        self.P.op("dve", lambda E: E.reciprocal(o.ap, a.ap), reads=[a.k], writes=[o.k])

    def scan(self, o, d0, d1, init):
        self.P.op("dve", lambda E: E.tensor_tensor_scan(o.ap, d0.ap, d1.ap, self._s(init), ALU.mult, ALU.add),
                  reads=self._k(d0, d1, init), writes=[o.k])

    def mm(self, o, lhsT, rhs, start=True, stop=True):
        self.P.op("pe", lambda E: E.matmul(o.ap, lhsT=lhsT.ap, rhs=rhs.ap, start=start, stop=stop),
                  reads=[lhsT.k, rhs.k], writes=[o.k])

    def tr(self, o, a, ident):
        self.P.op("pe", lambda E: E.transpose(o.ap, a.ap, ident.ap), reads=[a.k, ident.k], writes=[o.k])

    def ld(self, o, dram_ap, **kw):
        self.P.dma(o.ap, dram_ap, writes=[o.k], **kw)

    def stq(self, dram_ap, a, **kw):
        self.P.dma(dram_ap, a.ap, reads=[a.k], **kw)


def pv_layout():
    names = []
    for g in range(4):
        for j in range(4):
            names.append(("lru_cw", j, g))
        names.append(("lru_cb", g))
        for d in range(2):
            names.append(("lru_ba", d, g))
            names.append(("lru_bi", d, g))
            names.append(("lru_lam", d, g))
    for h in range(4):
        names.append(("hg_lb0", h))
        names.append(("hg_lb1", h))
        names.append(("hg_ng", h))
        names.append(("ml_ng", h))
        for d in range(2):
            names.append(("ml_bi", d, h))
            names.append(("ml_bf", d, h))
    for d in range(2):
        for c in range(4):
            for nm in ("mu_r", "mu_k", "mu_v", "w0", "a0", "k_k", "k_a", "r_k", "ln_w", "ln_b"):
                names.append((nm, d, c))
        names.append(("mu_wd", d))
        names.append(("mu_ad", d))
    return {n: i for i, n in enumerate(names)}


PVL = pv_layout()
NPV = len(PVL)
TB = 1024


def blocks(d):
    bl = [(0, LC)] + [(LC + j * TB, LC + (j + 1) * TB) for j in range(L // TB)]
    if d == 0:
        return [(lo, hi, False) for (lo, hi) in bl]
    lat = bl[1:]
    return [(0, LC, True)] + [(lo, hi, True) for (lo, hi) in reversed(lat)]


def rv(buf, n, rev, off=0):
    ap = buf.t[:, off:off + n]
    if rev:
        ap = ap[:, ::-1]
    return View(ap, buf.k)


def build(debug=False):
    nc = bass.Bass("TRN2", target_bir_lowering=False)

    def din(name, shape, dt=F32):
        return nc.dram_tensor(name, list(shape), dt, kind="ExternalInput").ap()

    def dscr(name, shape, dt=F32):
        return nc.dram_tensor(name, list(shape), dt, kind=("ExternalOutput" if (debug and name in ("YT", "H")) else "Internal")).ap()

    x_in = din("x", [L, D])
    ctx_in = din("ctx", [LC, D])
    cvec = din("cvec", [2, D])
    n1g = din("n1g", [2, D])
    n2g = din("n2g", [2, D])
    fing = din("fing", [D])
    modw = din("modw", [2, D, 6 * D])
    modb = din("modb", [2, 6 * D])
    w1 = din("w1", [2, D, DFF])
    w3 = din("w3", [2, D, DFF])
    w2 = din("w2", [2, DFF, D])
    inw = [din("inw0", [D, 4096]), din("inw1", [D, 3584])]
    outw = [din("outw0", [D, D]), din("outw1", [D, D])]
    pv = din("pv", [128, NPV])
    identd = din("identd", [128, 128])
    eyeflat = din("eyeflat", [128 * 128])
    lruw = din("lruw", [16, 128, 128])
    rw2 = din("rw2", [2, 64, 512])
    ra2 = din("ra2", [2, 64, 512])
    rg2 = din("rg2", [128, 512])
    bonesd = din("bonesd", [128, 128])
    masksd = din("masksd", [6, 128, 128])
    out_d = nc.dram_tensor("out", [L, D], F32, kind="ExternalOutput").ap()
    TBLD = [dscr("TBL0", [NPOS, 2560]), dscr("TBL1", [NPOS, 2560])]
    VSD = [dscr("VS0", [512, NPOS]), dscr("VS1", [512, NPOS])]
    BND = [dscr("BN0", [512, NPOS]), dscr("BN1", [512, NPOS])]
    ORW = [dscr("ORW0", [512, NPOS]), dscr("ORW1", [512, NPOS])]

    H = dscr("H", [L, D])
    HC = dscr("HC", [LC, D])
    PT = dscr("PT", [4096, NPOS])
    YT = dscr("YT", [D, NPOS], BF16)
    VT = dscr("VT", [D, NPOS], BF16)
    AT = dscr("AT", [DFF, NPOS], BF16)
    OT = [dscr("OT0", [512, NPOS]), dscr("OT1", [512, NPOS])]
    MODS = dscr("MODS", [2, 6 * D])

    with ExitStack() as st:
        K = KB(nc, st)
        P = K.P
        pvt = K.sb([128, NPV], name="pvt")
        K.ld(pvt[:, :], pv[:, :])
        idf = K.sb([128, 128], name="idf")
        K.ld(idf[:, :], identd[:, :])
        idb = K.sb([128, 128], BF16, name="idb")
        K.cp(idb[:, :], idf[:, :])
        onesf = K.sb([128, 128], name="onesf")
        K.memset(onesf[:, :], 1.0)
        onesb = K.sb([128, 128], BF16, name="onesb")
        K.memset(onesb[:, :], 1.0)
        WA1 = K.sb([128, 16384], BF16, name="WA1")
        WA2 = K.sb([128, 16384], BF16, name="WA2")
        SA = K.sb([128, 4096], F32, name="SA")
        G4 = [K.sb([128, D], name="g4_%d" % i) for i in range(6)]
        G2 = [K.sb([128, D], BF16, name="g2_%d" % i) for i in range(2)]
        T8 = K.sb([128, 8, 512], BF16, name="T8")
        T2 = [K.sb([128, 8, 128], BF16, name="t2_%d" % i) for i in range(2)]
        ATt = K.sb([128, 22, 128], BF16, name="ATt")
        ATt2 = K.sb([128, 22, 128], BF16, name="ATt2")
        T2X = [K.sb([128, 8, 128], BF16, name="t2x_%d" % i) for i in range(2)]
        STG = [K.sb([128, 512], name="stg%d" % i) for i in range(3)]
        ASTG = [K.sb([128, 512], BF16, name="astg%d" % i) for i in range(3)]
        MX = [K.sb([128, TB + 8], name="mx%d" % i) for i in range(9)]
        MXB = [K.sb([128, TB], BF16, name="mxb%d" % i) for i in range(2)]
        SST = K.sb([128, 128], name="SST")
        NST = K.sb([128, 1], name="NST")
        ssb = K.sb([128, 1], name="ssb")
        smalls = K.sb([128, 16], name="smalls")
        LW = K.sb([128, 16, 128], name="LW")
        pbank = [K.ps([128, 512], name="pb%d" % i) for i in range(7)]
        pst = K.ps([128, 8, 128], BF16, name="pst")

        def pcol(*name):
            i = PVL[tuple(name)]
            return pvt[:, i:i + 1]

        def hrows(layer, seg, t0, n, src_override=None):
            if seg == 0:
                src = ctx_in if layer == 0 else HC
                return src[t0:t0 + n, :]
            src = x_in if layer == 0 else H
            if layer % 2 == 0:
                return src[t0:t0 + n, :]
            c = t0 // ROWS
            return src.rearrange("(r c) d -> c r d", c=GRID_W)[c]

        def hrows_out(layer, seg, t0, n, final=False):
            if seg == 0:
                return HC[t0:t0 + n, :]
            dst = out_d if final else H
            if layer % 2 == 0:
                return dst[t0:t0 + n, :]
            c = t0 // ROWS
            return dst.rearrange("(r c) d -> c r d", c=GRID_W)[c]

        SEGS = [(0, 0, LC), (1, LC, L)]

        def winv(c0, c1):
            if c1 <= 2048:
                return View(WA1.t[:, :].rearrange("p (k n) -> p k n", k=8)[:, :, c0:c1], WA1.k)
            return View(WA2.t[:, :].rearrange("p (k n) -> p k n", k=8)[:, :, c0 - 2048:c1 - 2048], WA2.k)

        def norm_mod(hin, A, B, outb, junk):
            K.act(junk, hin, AF.Square, accum=ssb[:, :])
            K.act(ssb[:, :], ssb[:, :], AF.Sqrt, scale=1.0 / D, bias=EPS)
            K.recip(ssb[:, :], ssb[:, :])
            K.stt(junk, hin, ssb[:, 0:1], A, ALU.mult, ALU.mult)
            K.tt(outb, junk, B, ALU.add)

        def load_mod(buf, layer, seg, i, gsrc=None):
            s = 1 if seg == 0 else 0
            K.ld(buf[:, :], MODS[s, i * D:(i + 1) * D].partition_broadcast(128))
            if gsrc is not None:
                K.ld(G4[5][:, :], gsrc.partition_broadcast(128))
                K.stt(buf[:, :], buf[:, :], 1.0, G4[5][:, :], ALU.add, ALU.mult)

        for layer in range(2):
            last = layer == 1
            ctile = smalls
            P.dma(smalls.t[:, 0:16].rearrange("p (s k) -> p s k", s=2), cvec.rearrange("s (k p) -> p s k", p=128),
                  writes=[smalls.k], q="sp", allow_slow_non_contiguous=True)
            K.act(smalls[:, 0:16], smalls[:, 0:16], AF.Silu)
            clhs = G4
            for s in range(2):
                for k in range(8):
                    K.cp(G4[s * 2 + k // 4][:, (k % 4) * 128:(k % 4 + 1) * 128] if False else
                         View(G4[s].t[:, k * 128:(k + 1) * 128], G4[s].k),
                         View(smalls.t[:, s * 8 + k:s * 8 + k + 1].to_broadcast([128, 128]), smalls.k))
            mwv = SA.t[:, :].rearrange("p (k n) -> p k n", k=8)
            for g in range(12):
                K.P.dma(mwv, modw[layer].rearrange("(k p) n -> p k n", p=128)[:, :, g * 512:(g + 1) * 512], writes=[SA.k])
                K.P.dma(STG[0].t[:, :], modb[layer, g * 512:(g + 1) * 512].partition_broadcast(128), writes=[STG[0].k])
                for s in range(2):
                    pb_ = pbank[s]
                    for k in range(8):
                        K.mm(pb_[:, :], View(G4[s].t[:, k * 128:(k + 1) * 128], G4[s].k), SA.v(mwv[:, k, :]), start=(k == 0), stop=(k == 7))
                    K.tt(STG[1 + s][:, :], pb_[:, :], STG[0][:, :], ALU.add)
                    K.stq(MODS[s:s + 1, g * 512:(g + 1) * 512], STG[1 + s][0:1, :])
            P.barrier()

            ncols = 4096 if layer == 0 else 3584
            nch = ncols // 128
            for g in range(ncols // 512):
                stg = SA.t[:, :].rearrange("p (k n) -> p k n", k=8)
                K.P.dma(stg, inw[layer].rearrange("(k p) n -> p k n", p=128)[:, :, g * 512:(g + 1) * 512], writes=[SA.k])
                K.cp(winv(g * 512, (g + 1) * 512), SA.v(stg))
            ci = 0
            for (seg, poff, slen) in SEGS:
                load_mod(G4[1], layer, seg, 1, n1g[layer])
                load_mod(G4[2], layer, seg, 0)
                bs = min(512, slen)
                for b0 in range(0, slen, bs):
                    for t in range(bs // 128):
                        hb_ = (G4[0], G4[4])[t % 2]
                        ub_ = G2[t % 2]
                        K.ld(hb_[:, :], hrows(layer, seg, b0 + t * 128, 128))
                        norm_mod(hb_[:, :], G4[1][:, :], G4[2][:, :], ub_[:, :], G4[3][:, :])
                        for k in range(8):
                            K.tr(pst[:, k, :], ub_[:, k * 128:(k + 1) * 128], idb[:, :])
                        K.cp(T8[:, :, t * 128:(t + 1) * 128], pst[:, :, :], e="act")
                    for c in range(nch):
                        pb_ = pbank[c % 6]
                        wv = winv(c * 128, (c + 1) * 128)
                        for k in range(8):
                            K.mm(pb_[:, 0:bs], View(wv.ap[:, k, :], wv.k), T8[:, k, 0:bs], start=(k == 0), stop=(k == 7))
                        sg = STG[ci % 3]
                        ci += 1
                        K.cp(sg[:, 0:bs], pb_[:, 0:bs], e=("act" if c % 2 else "dve"))
                        K.stq(PT[c * 128:(c + 1) * 128, poff + b0:poff + b0 + bs], sg[:, 0:bs])
            P.barrier()

            WAf = WA1.t[:, :].bitcast(F32)
            WBf = WA2.t[:, :].bitcast(F32)
            RX = [Buf(WAf[:, i * 520:(i + 1) * 520]) for i in range(15)]
            _o = [0]

            def cw(n):
                b = Buf(WBf[:, _o[0]:_o[0] + n])
                _o[0] += n
                return b
            MSK = cw(768)
            MK4 = [cw(512), cw(512)]
            RS = cw(1024)
            TMA = [cw(640), cw(640)]
            STp = [cw(128) for _ in range(4)]
            CT = cw(128)
            NBb = cw(128)
            SSh = cw(128)
            Dt = cw(128)
            St = cw(128)
            VK = cw(256)
            KW = cw(128)
            JK = cw(128)
            Am = cw(128)
            ccol = cw(8)
            wcol = cw(8)
            ebT = cw(8)
            GTc = cw(8)
            Dt2 = [Dt, cw(128)]
            St2 = [St, cw(128)]
            VK2 = [VK, cw(256)]
            KW2 = [KW, cw(128)]
            JK2 = [JK, cw(128)]
            JD2 = [cw(128), cw(128)]
            Am2 = [Am, cw(128)]
            cc2 = [ccol, cw(8)]
            wc2 = [wcol, cw(8)]
            eb2 = [ebT, cw(8)]
            MSK3 = MSK.t[:, :].rearrange("p (m q) -> p m q", m=6)
            K.P.dma(MSK3, masksd.rearrange("m p q -> p m q"), writes=[MSK.k])

            def msk(i):
                return View(MSK3[:, i, :], MSK.k)
            for d_ in range(2):
                for j_ in range(4):
                    K.cp(MK4[d_][:, j_ * 128:(j_ + 1) * 128], msk((4 if j_ % 2 == 0 else 2) + d_))

            def rst_mask(T):
                K.memset(RS[:, 0:1024], 1.0)
                K.memset(View(RS.t[:, 0:1024].rearrange("p (c t) -> p c t", t=T)[:, :, 0:1], RS.k), 0.0)

            def red(o, a):
                K.P.op("dve", lambda E: E.reduce_sum(o.ap, a.ap, AX.X), reads=[a.k], writes=[o.k])

            def merge(h, rows0_y, gate_rows0, gate_func, ngcol, lo0, hi0):
                for it_, lo in enumerate(range(lo0, hi0, 512)):
                    n = min(512, hi0 - lo)
                    par_ = it_ % 2
                    BX = MX if par_ == 0 else RX
                    K.ld(BX[0][:, 0:n], OT[0][h * 128:(h + 1) * 128, lo:lo + n])
                    K.ld(BX[1][:, 0:n], OT[1][h * 128:(h + 1) * 128, lo:lo + n])
                    K.ld(BX[2][:, 0:n], PT[gate_rows0 + h * 128:gate_rows0 + (h + 1) * 128, lo:lo + n])
                    K.tt(BX[0][:, 0:n], BX[0][:, 0:n], BX[1][:, 0:n], ALU.add)
                    K.tt(BX[1][:, 0:n], BX[0][:, 0:n], BX[0][:, 0:n], ALU.mult)
                    K.mm(pbank[3 * par_][:, 0:n], onesf[:, :], BX[1][:, 0:n])
                    K.act(BX[1][:, 0:n], pbank[3 * par_][:, 0:n], AF.Sqrt, scale=1.0 / 128, bias=EPS)
                    K.recip(BX[1][:, 0:n], BX[1][:, 0:n])
                    K.stt(BX[0][:, 0:n], BX[0][:, 0:n], ngcol, BX[1][:, 0:n], ALU.mult, ALU.mult)
                    K.act(BX[2][:, 0:n], BX[2][:, 0:n], gate_func)
                    K.tt(MXB[par_][:, 0:n], BX[0][:, 0:n], BX[2][:, 0:n], ALU.mult)
                    K.stq(YT[rows0_y + h * 128:rows0_y + (h + 1) * 128, lo:lo + n], MXB[par_][:, 0:n])


            if layer == 0:
                RWs = Buf(LW.t[0:64, :, :].rearrange("p a o -> p (a o)").rearrange("p (x y z) -> p x y z", x=2, y=2))
                RWs.k = LW.k
                RG2s = Buf(G4[5].t[:, 0:512])
                RG2s.k = G4[5].k
                bones = Buf(G4[4].t[:, 0:128])
                bones.k = G4[4].k
                K.ld(bones[:, :], bonesd[:, :])
                for d in range(2):
                    K.ld(RWs[:, 0, d, :], rw2[d])
                    K.ld(RWs[:, 1, d, :], ra2[d])
                K.ld(RG2s[:, :], rg2[:, :])

                def shifted(dst, row0, nrows, d, lo, hi, mucol):
                    n = hi - lo
                    slo, shi = (0, LC) if lo < LC else (LC, NPOS)
                    xh = MX[5]
                    K.memset(xh[0:nrows, 0:n + 2], 0.0)
                    a0_ = max(lo - 1, slo)
                    a1_ = min(hi + 1, shi)
                    K.ld(View(xh.t[0:nrows, a0_ - (lo - 1):a1_ - (lo - 1)], xh.k), PT[row0:row0 + nrows, a0_:a1_])
                    cur = xh[0:nrows, 1:1 + n]
                    prev = xh[0:nrows, 0:n] if d == 0 else xh[0:nrows, 2:2 + n]
                    K.tt(dst[0:nrows, 0:n], prev, cur, ALU.subtract)
                    K.stt(dst[0:nrows, 0:n], dst[0:nrows, 0:n], mucol, cur, ALU.mult, ALU.add)


                rst_mask(64)
                PADS = [MX[0], MX[1], MX[2], MX[3], MX[4], MX[7], MX[8]]
                for pb_ in PADS:
                    K.memset(pb_[:, 0:1024], 0.0)

                def pad3(buf):
                    return buf.t[:, 0:1024].rearrange("p (c t) -> p c t", t=128)

                def padq(buf, q):
                    return View(pad3(buf)[:, q, :], buf.k)

                def fill_pad(buf, src, nck, mul_bc=None):
                    for hh in range(2):
                        pr = slice(hh * 64, hh * 64 + 64)
                        o = View(pad3(buf)[pr, 0:nck, hh * 64:hh * 64 + 64], buf.k)
                        s3 = View(src.t[pr, 0:nck * 64].rearrange("p (c t) -> p c t", t=64), src.k)
                        if mul_bc is None:
                            K.cp(o, s3, e=("act" if hh else "dve"))
                        else:
                            K.tt(o, s3, View(mul_bc.t[pr, 0:nck].unsqueeze(2).to_broadcast([64, nck, 64]), mul_bc.k), ALU.mult)

                def blocksR(d):
                    bl = [(LC + j * 512, LC + (j + 1) * 512) for j in range(L // 512)]
                    if d == 0:
                        return [(0, LC, False)] + [(lo, hi, False) for (lo, hi) in bl]
                    return [(0, LC, True)] + [(lo, hi, True) for (lo, hi) in reversed(bl)]

                PA, PR, PB, PK, PV, PBG, PKG = PADS
                cnt = 0
                for d in range(2):
                    for c in range(4):
                        K.memset(STp[c][:, :], 0.0)
                    for (lo, hi, rev) in blocksR(d):
                        n = hi - lo
                        nck = n // 64
                        shifted(RX[10], 1536 + 64 * d, 64, d, lo, hi, View(pvt.t[0:64, PVL[("mu_wd", d)]:PVL[("mu_wd", d)] + 1], pvt.k))
                        K.act(RX[10][0:64, 0:n], RX[10][0:64, 0:n], AF.Tanh)
                        shifted(RX[11], 1664 + 64 * d, 64, d, lo, hi, View(pvt.t[0:64, PVL[("mu_ad", d)]:PVL[("mu_ad", d)] + 1], pvt.k))
                        for c in range(4):
                            cs = slice(c * 128, (c + 1) * 128)
                            shifted(RX[0], c * 128, 128, d, lo, hi, pcol("mu_r", d, c))
                            shifted(RX[1], 512 + c * 128, 128, d, lo, hi, pcol("mu_k", d, c))
                            shifted(RX[2], 1024 + c * 128, 128, d, lo, hi, pcol("mu_v", d, c))
                            K.mm(pbank[0][:, 0:n], RWs[:, 0, d, cs], RX[10][0:64, 0:n])
                            K.mm(pbank[1][:, 0:n], RWs[:, 1, d, cs], RX[11][0:64, 0:n])
                            K.act(RX[3][:, 0:n], pbank[0][:, 0:n], AF.Sigmoid, bias=pcol("w0", d, c))
                            K.ts(RX[3][:, 0:n], RX[3][:, 0:n], -0.6065306597126334, ALU.mult)
                            K.act(RX[4][:, 0:n], pbank[1][:, 0:n], AF.Sigmoid, bias=pcol("a0", d, c))
                            K.ts(RX[5][:, 0:n], RX[1][:, 0:n], pcol("k_k", d, c), ALU.mult)
                            K.tt(RX[9][:, 0:n], RX[5][:, 0:n], RX[5][:, 0:n], ALU.mult)
                            K.mm(pbank[2][:, 0:n], bones[:, :], RX[9][:, 0:n])
                            K.act(RX[9][:, 0:n], pbank[2][:, 0:n], AF.Sqrt)
                            K.ts(RX[9][:, 0:n], RX[9][:, 0:n], 1e-12, ALU.max)
                            K.recip(RX[9][:, 0:n], RX[9][:, 0:n])
                            K.tt(RX[5][:, 0:n], RX[5][:, 0:n], RX[9][:, 0:n], ALU.mult)
                            K.ts(RX[9][:, 0:n], RX[4][:, 0:n], -1.0, ALU.add, pcol("k_a", d, c), ALU.mult)
                            K.stt(RX[1][:, 0:n], RX[9][:, 0:n], 1.0, RX[1][:, 0:n], ALU.add, ALU.mult)
                            K.tt(RX[4][:, 0:n], RX[4][:, 0:n], RX[5][:, 0:n], ALU.mult)
                            K.stt(RX[9][:, 0:n], RX[0][:, 0:n], pcol("r_k", d, c), RX[1][:, 0:n], ALU.mult, ALU.mult)
                            K.mm(pbank[3][:, 0:n], bones[:, :], RX[9][:, 0:n])
                            K.tt(RX[9][:, 0:n], pbank[3][:, 0:n], RX[2][:, 0:n], ALU.mult)
                            K.stq(BND[d][cs, lo:hi], RX[9][:, 0:n])
                            K.scan(rv(RX[6], n, rev), RS[:, 0:n], rv(RX[3], n, rev), 0.0)
                            K.act(RX[7][:, 0:n], RX[6][:, 0:n], AF.Exp)
                            K.tt(RX[0][:, 0:n], RX[0][:, 0:n], RX[7][:, 0:n], ALU.mult)
                            K.tt(RX[9][:, 0:n], RX[6][:, 0:n], RX[3][:, 0:n], ALU.subtract)
                            K.act(RX[9][:, 0:n], RX[9][:, 0:n], AF.Exp)
                            K.stt(RX[8][:, 0:n], RX[5][:, 0:n], -1.0, RX[9][:, 0:n], ALU.mult, ALU.mult)
                            K.act(RX[9][:, 0:n], RX[6][:, 0:n], AF.Exp, scale=-1.0)
                            K.tt(RX[4][:, 0:n], RX[4][:, 0:n], RX[9][:, 0:n], ALU.mult)
                            K.tt(RX[1][:, 0:n], RX[1][:, 0:n], RX[9][:, 0:n], ALU.mult)
                            lastoff = 0 if rev else 63
                            K.cp(GTc[:, 0:nck], View(RX[7].t[:, 0:n].rearrange("p (c t) -> p c t", t=64)[:, :, lastoff], RX[7].k))
                            fill_pad(PA, RX[8], nck)
                            fill_pad(PR, RX[0], nck)
                            fill_pad(PB, RX[4], nck)
                            fill_pad(PK, RX[1], nck)
                            fill_pad(PV, RX[2], nck)
                            fill_pad(PBG, RX[4], nck, mul_bc=GTc)
                            fill_pad(PKG, RX[1], nck, mul_bc=GTc)
                            qs_ = list(range(nck))
                            if rev:
                                qs_.reverse()
                            ST = STp[c]
                            def mk_stages(q, p_):
                                sc = STG[p_]
                                g = G4[p_]
                                w = G4[2 + p_]
                                tm = TMA[p_]
                                b0, b1, b2 = pbank[3 * p_], pbank[3 * p_ + 1], pbank[3 * p_ + 2]
                                bs = pbank[6]
                                A_, R_, B_, K_, V_, BG_, KG_ = [padq(x, q) for x in PADS]
                                st = []

                                def s_scores():
                                    K.mm(b0[:, 0:128], B_, A_)
                                    K.mm(b0[:, 128:256], B_, R_)
                                    K.mm(b0[:, 256:384], K_, A_)
                                    K.mm(b0[:, 384:512], K_, R_)
                                    K.mm(b1[:, 0:128], A_, B_)
                                    K.mm(b1[:, 128:256], A_, idf[:, :])
                                    K.mm(b2[:, 0:128], V_, idf[:, :])
                                    K.mm(b2[:, 128:256], BG_, idf[:, :])
                                    K.mm(b2[:, 256:384], KG_, idf[:, :])
                                st.append(s_scores)

                                def s_evac1():
                                    K.tt(sc[:, :], b0[:, :], MK4[d][:, :], ALU.mult)
                                    K.tt(g[:, 0:128], b1[:, 0:128], msk(4 + (1 - d)), ALU.mult)
                                    K.cp(g[:, 128:256], b1[:, 128:256], e="act")
                                    K.cp(tm[:, 0:384], b2[:, 0:384], e="act")
                                st.append(s_evac1)

                                def s_av():
                                    K.mm(b1[:, 256:384], sc[:, 256:384], tm[:, 0:128])
                                    K.mm(b0[:, 0:128], sc[:, 0:128], g[:, 0:128])
                                    K.mm(b0[:, 128:256], g[:, 0:128], sc[:, 0:128])
                                st.append(s_av)

                                def s_evac2():
                                    K.cp(g[:, 256:384], b1[:, 256:384])
                                    K.cp(w[:, 0:256], b0[:, 0:256], e="act")
                                    K.tt(w[:, 256:384], idf[:, :], sc[:, 0:128], ALU.add)
                                st.append(s_evac2)
                                for k in range(1, 6):
                                    wi0 = ((k - 1) % 2) * 384
                                    wo0 = (k % 2) * 384
                                    if k < 5:
                                        def s_mm(wi0=wi0):
                                            K.mm(b0[:, 128:384], w[:, wi0:wi0 + 128], w[:, wi0 + 128:wi0 + 384])
                                            K.mm(b0[:, 0:128], w[:, wi0 + 128:wi0 + 256], w[:, wi0:wi0 + 128])

                                        def s_ev(wi0=wi0, wo0=wo0):
                                            K.cp(w[:, wo0:wo0 + 256], b0[:, 0:256], e="act")
                                            K.tt(w[:, wo0 + 256:wo0 + 384], w[:, wi0 + 256:wi0 + 384], b0[:, 256:384], ALU.add)
                                    else:
                                        def s_mm(wi0=wi0):
                                            K.mm(b0[:, 256:384], w[:, wi0:wi0 + 128], w[:, wi0 + 256:wi0 + 384])

                                        def s_ev(wi0=wi0, wo0=wo0):
                                            K.tt(g[:, 384:512], w[:, wi0 + 256:wi0 + 384], b0[:, 256:384], ALU.add)
                                    st.append(s_mm)
                                    st.append(s_ev)

                                def s_aw():
                                    K.mm(b1[:, 0:256], g[:, 384:512], g[:, 128:384])
                                st.append(s_aw)

                                def s_aw_ev():
                                    K.cp(tm[:, 384:640], b1[:, 0:256], e="act")
                                st.append(s_aw_ev)

                                def s_ghq():
                                    K.mm(b1[:, 384:512], tm[:, 384:512], tm[:, 128:256])
                                    K.mm(b2[:, 384:512], tm[:, 128:256], tm[:, 512:640], start=True, stop=False)
                                    K.mm(b2[:, 384:512], tm[:, 256:384], tm[:, 0:128], start=False, stop=True)
                                    K.mm(b1[:, 256:384], tm[:, 384:512], sc[:, 128:256])
                                st.append(s_ghq)

                                def s_ghq_ev():
                                    K.stt(g[:, 512:640], idf[:, :], GTc[:, q:q + 1], b1[:, 384:512], ALU.mult, ALU.add)
                                    K.cp(g[:, 640:768], b2[:, 384:512], e="act")
                                    K.tt(g[:, 768:896], b1[:, 256:384], R_, ALU.add)
                                st.append(s_ghq_ev)

                                def s_seq():
                                    o0 = p_ * 256
                                    K.mm(bs[:, o0:o0 + 128], ST[:, :], g[:, 768:896], start=True, stop=False)
                                    K.mm(bs[:, o0:o0 + 128], tm[:, 512:640], sc[:, 128:256], start=False, stop=False)
                                    K.mm(bs[:, o0:o0 + 128], tm[:, 0:128], sc[:, 384:512], start=False, stop=True)
                                    K.mm(bs[:, o0 + 128:o0 + 256], g[:, 512:640], ST[:, :])
                                    K.tt(ST[:, :], bs[:, o0 + 128:o0 + 256], g[:, 640:768], ALU.add)
                                    K.cp(g[:, 896:1024], bs[:, o0:o0 + 128], e="act")
                                    K.tt(RX[12][:, q * 64:(q + 1) * 64], g[:, 896:960], g[:, 960:1024], ALU.add)
                                return st, s_seq

                            for qi in range(0, nck, 2):
                                sa, seqa = mk_stages(qs_[qi], 0)
                                sb2, seqb = mk_stages(qs_[qi + 1], 1)
                                for fa, fb in zip(sa, sb2):
                                    fa()
                                    fb()
                                seqa()
                                seqb()
                            K.stq(ORW[d][cs, lo:hi], RX[12][:, 0:n])
                P.barrier()
                for c in range(4):
                    cs = slice(c * 128, (c + 1) * 128)
                    for it_, lo in enumerate(range(0, NPOS, 512)):
                        n = min(512, NPOS - lo)
                        par_ = it_ % 2
                        BX = MX if par_ == 0 else RX
                        for d in range(2):
                            K.ld(BX[0][:, 0:n], ORW[d][cs, lo:lo + n])
                            K.mm(pbank[3 * par_ + 0][:, 0:n], bones[:, :], BX[0][:, 0:n])
                            K.stt(BX[0][:, 0:n], pbank[3 * par_ + 0][:, 0:n], -1.0 / 64, BX[0][:, 0:n], ALU.mult, ALU.add)
                            K.tt(BX[1][:, 0:n], BX[0][:, 0:n], BX[0][:, 0:n], ALU.mult)
                            K.mm(pbank[3 * par_ + 1][:, 0:n], bones[:, :], BX[1][:, 0:n])
                            K.act(BX[1][:, 0:n], pbank[3 * par_ + 1][:, 0:n], AF.Sqrt, scale=1.0 / 64, bias=64e-5)
                            K.recip(BX[1][:, 0:n], BX[1][:, 0:n])
                            K.stt(BX[0][:, 0:n], BX[0][:, 0:n], pcol("ln_w", d, c), BX[1][:, 0:n], ALU.mult, ALU.mult)
                            K.ld(BX[2][:, 0:n], BND[d][cs, lo:lo + n])
                            K.stt(BX[3 + d][:, 0:n], BX[0][:, 0:n], pcol("ln_b", d, c), BX[2][:, 0:n], ALU.add, ALU.add)
                        K.tt(BX[3][:, 0:n], BX[3][:, 0:n], BX[4][:, 0:n], ALU.add)
                        K.ld(BX[5][:, 0:n], PT[1792:1920, lo:lo + n])
                        K.act(BX[5][:, 0:n], BX[5][:, 0:n], AF.Sigmoid)
                        K.mm(pbank[3 * par_ + 2][:, 0:n], RG2s[:, cs], BX[5][:, 0:n])
                        K.tt(MXB[par_][:, 0:n], BX[3][:, 0:n], pbank[3 * par_ + 2][:, 0:n], ALU.mult)
                        K.stq(YT[cs, lo:lo + n], MXB[par_][:, 0:n])
                P.barrier()
                rst_mask(128)
                for d in range(2):
                    for h in range(4):
                        K.memset(CT[:, :], 0.0)
                        K.memset(NBb[:, :], 0.0)
                        for (lo, hi, rev) in blocks(d):
                            n = hi - lo
                            K.ld(MX[0][:, 0:n], PT[3968 + 8 + d * 4 + h, lo:hi].partition_broadcast(128))
                            K.act(MX[0][:, 0:n], MX[0][:, 0:n], AF.Sigmoid, bias=pcol("ml_bf", d, h))
                            K.act(MX[0][:, 0:n], MX[0][:, 0:n], AF.Ln)
                            K.scan(rv(MX[6], n, rev), RS[:, 0:n], rv(MX[0], n, rev), 0.0)
                            K.ld(MX[1][:, 0:n], PT[3968 + d * 4 + h, lo:hi].partition_broadcast(128))
                            K.stt(MX[1][:, 0:n], MX[1][:, 0:n], pcol("ml_bi", d, h), MX[6][:, 0:n], ALU.add, ALU.subtract)
                            K.ld(MX[2][:, 0:n], PT[1920 + h * 128:1920 + (h + 1) * 128, lo:hi])
                            K.ts(MX[2][:, 0:n], MX[2][:, 0:n], 128.0 ** -0.5, ALU.mult)
                            K.act(MX[7][:, 0:n], MX[6][:, 0:n], AF.Exp)
                            K.tt(MX[7][:, 0:n], MX[7][:, 0:n], MX[2][:, 0:n], ALU.mult)
                            K.ld(MX[3][:, 0:n], PT[2432 + h * 128:2432 + (h + 1) * 128, lo:hi])
                            K.ld(MX[4][:, 0:n], PT[2944 + h * 128:2944 + (h + 1) * 128, lo:hi])
                            qs_ = list(range(n // 128))
                            if rev:
                                qs_.reverse()
                            def ml_stages(q, p_):
                                cs = slice(q * 128, (q + 1) * 128)
                                lastc = q * 128 if rev else q * 128 + 127
                                Dt_, St_, VK_, KW_, JK_, JD_ = Dt2[p_], St2[p_], VK2[p_], KW2[p_], JK2[p_], JD2[p_]
                                cc_, wc_, eb_ = cc2[p_], wc2[p_], eb2[p_]
                                a0, a1, a2 = pbank[3 * p_], pbank[3 * p_ + 1], pbank[3 * p_ + 2]

                                def ind():
                                    K.mm(a0[:, 0:128], MX[3][:, cs], MX[2][:, cs])
                                    K.mm(a1[:, 0:128], MX[4][:, cs], idf[:, :])
                                    K.mm(a1[:, 128:256], MX[3][:, cs], idf[:, :])
                                    K.tt(JK_[:, :], MX[1][:, cs], idf[:, :], ALU.mult)
                                    red(cc_[:, 0:1], JK_[:, :])
                                    K.act(Dt_[:, :], MX[6][:, cs], AF.Exp, bias=cc_[:, 0:1])
                                    K.tt(Dt_[:, :], Dt_[:, :], msk(d), ALU.mult)
                                    K.tt(St_[:, :], a0[:, 0:128], Dt_[:, :], ALU.mult)
                                    K.cp(VK_[:, :], a1[:, 0:256], e="act")
                                    K.act(wc_[:, 0:1], cc_[:, 0:1], AF.Exp, bias=MX[6][:, lastc:lastc + 1])
                                    K.act(eb_[:, 0:1], MX[6][:, lastc:lastc + 1], AF.Exp)
                                    K.ts(KW_[:, :], VK_[:, 128:256], wc_[:, 0:1], ALU.mult)

                                def seq():
                                    K.mm(a0[:, 128:256], VK_[:, 0:128], St_[:, :], start=True, stop=False)
                                    K.mm(a0[:, 128:256], CT[:, :], MX[7][:, cs], start=False, stop=True)
                                    K.mm(a1[:, 256:384], onesf[:, :], St_[:, :], start=True, stop=False)
                                    K.mm(a1[:, 256:384], NBb[:, :], MX[7][:, cs], start=False, stop=True)
                                    K.mm(a2[:, 0:128], KW_[:, :], VK_[:, 0:128])
                                    K.mm(a2[:, 128:256], KW_[:, :], onesf[:, :])
                                    K.stt(CT[:, :], CT[:, :], eb_[:, 0:1], a2[:, 0:128], ALU.mult, ALU.add)
                                    K.stt(NBb[:, :], NBb[:, :], eb_[:, 0:1], a2[:, 128:256], ALU.mult, ALU.add)
                                    K.act(JD_[:, :], a1[:, 256:384], AF.Abs)
                                    K.ts(JD_[:, :], JD_[:, :], 1.0, ALU.max)
                                    K.recip(JD_[:, :], JD_[:, :])
                                    K.tt(MX[8][:, cs], a0[:, 128:256], JD_[:, :], ALU.mult)
                                return ind, seq

                            for qi in range(0, len(qs_), 2):
                                ia, sa_ = ml_stages(qs_[qi], 0)
                                ib, sb2_ = ml_stages(qs_[qi + 1], 1)
                                ia()
                                ib()
                                sa_()
                                sb2_()
                            K.stq(OT[d][h * 128:(h + 1) * 128, lo:hi], MX[8][:, 0:n])
                P.barrier()
                for h in range(4):
                    merge(h, 512, 3456, AF.Sigmoid, pcol("ml_ng", h), 0, NPOS)
            else:
                rst_mask(64)
                for h in range(4):
                    K.tt(smalls[:, 0:1], pcol("hg_lb1", h), pcol("hg_lb0", h), ALU.subtract)
                    K.act(smalls[:, 0:1], smalls[:, 0:1], AF.Sigmoid)
                    K.ts(smalls[:, 1:2], smalls[:, 0:1], -1.0, ALU.mult, 1.0, ALU.add)
                    for d in range(2):
                        K.memset(SSh[:, :], 0.0)
                        for (lo, hi, rev) in blocks(d):
                            n = hi - lo
                            nsub = n // 64
                            K.ld(MX[0][:, 0:n], PT[512 + d * 512 + h * 128:512 + d * 512 + (h + 1) * 128, lo:hi])
                            K.act(MX[0][:, 0:n], MX[0][:, 0:n], AF.Sigmoid)
                            K.ts(MX[0][:, 0:n], MX[0][:, 0:n], smalls[:, 1:2], ALU.mult, smalls[:, 0:1], ALU.add)
                            K.act(MX[1][:, 0:n], MX[0][:, 0:n], AF.Ln)
                            K.scan(rv(MX[2], n, rev), RS[:, 0:n], rv(MX[1], n, rev), 0.0)
                            K.ts(MX[3][:, 0:n], MX[0][:, 0:n], -1.0, ALU.mult, 1.0, ALU.add)
                            K.act(MX[4][:, 0:n], MX[2][:, 0:n], AF.Exp)
                            K.ld(MX[5][:, 0:n], PT[h * 128:(h + 1) * 128, lo:hi])
                            K.act(MX[5][:, 0:n], MX[5][:, 0:n], AF.Silu)
                            K.tt(MX[5][:, 0:n], MX[5][:, 0:n], MX[4][:, 0:n], ALU.mult)
                            K.cp(MXB[0][:, 0:n], MX[5][:, 0:n], e="act")
                            K.act(MX[6][:, 0:n], MX[2][:, 0:n], AF.Exp, scale=-1.0)
                            K.tt(MX[6][:, 0:n], MX[6][:, 0:n], MX[3][:, 0:n], ALU.mult)
                            K.cp(MXB[1][:, 0:n], MX[6][:, 0:n], e="act")
                            lastoff = 0 if rev else 63
                            e3 = MX[4].t[:, 0:n].rearrange("p (c t) -> p c t", t=64)[:, :, lastoff:lastoff + 1].to_broadcast([128, nsub, 64])
                            K.tt(View(G2[0].t[:, 0:n].rearrange("p (c t) -> p c t", t=64), G2[0].k),
                                 View(MX[6].t[:, 0:n].rearrange("p (c t) -> p c t", t=64), MX[6].k),
                                 View(e3, MX[4].k), ALU.mult)
                            K.ld(MX[8][:, 0:n], PT[1536 + h * 128:1536 + (h + 1) * 128, lo:hi])
                            K.cp(G2[1][:, 0:n], MX[8][:, 0:n])
                            qs_ = list(range(n // 128))
                            if rev:
                                qs_.reverse()
                            def hg_stages(q, p_):
                                cs = slice(q * 128, (q + 1) * 128)
                                JK_ = JK2[p_]
                                Am_ = Buf(T2X[p_].t[:, 0, :])
                                Am_.k = T2X[p_].k
                                VK_ = Buf(T2[p_].t[:, 0:2, :].rearrange("p a b -> p (a b)"))
                                VK_.k = T2[p_].k
                                a0, a1, a2 = pbank[3 * p_], pbank[3 * p_ + 1], pbank[3 * p_ + 2]

                                def ind():
                                    K.mm(a0[:, 0:128], MXB[1][:, cs], MXB[0][:, cs])
                                    K.mm(a1[:, 0:128], G2[1][:, cs], idb[:, :])
                                    K.mm(a1[:, 128:256], G2[0][:, cs], idb[:, :])
                                    K.tt(Am_[:, :], a0[:, 0:128], msk(2 + d), ALU.mult)
                                    K.cp(VK_[:, :], a1[:, 0:256], e="act")
                                    K.mm(a0[:, 128:256], VK_[:, 0:128], Am_[:, :])
                                    K.cp(JK_[:, :], a0[:, 128:256])

                                def seq():
                                    for sb_ in ((1, 0) if rev else (0, 1)):
                                        c0 = q * 128 + sb_ * 64
                                        pr = slice(sb_ * 64, sb_ * 64 + 64)
                                        K.mm(a2[:, sb_ * 64:sb_ * 64 + 64], SSh[:, :], MX[5][:, c0:c0 + 64])
                                        K.mm(a2[:, 128:256], VK_[pr, 128:256], VK_[pr, 0:128])
                                        K.stt(SSh[:, :], SSh[:, :], MX[4][:, c0 + lastoff:c0 + lastoff + 1], a2[:, 128:256], ALU.mult, ALU.add)
                                    K.tt(MX[1][:, cs], JK_[:, :], a2[:, 0:128], ALU.add)
                                return ind, seq

                            for qi in range(0, len(qs_), 2):
                                ia, sa_ = hg_stages(qs_[qi], 0)
                                ib, sb2_ = hg_stages(qs_[qi + 1], 1)
                                ia()
                                ib()
                                sa_()
                                sb2_()
                            K.stq(OT[d][h * 128:(h + 1) * 128, lo:hi], MX[1][:, 0:n])
                P.barrier()
                for h in range(4):
                    merge(h, 0, 2048, AF.Silu, pcol("hg_ng", h), LC, NPOS)
                P.barrier()
                K.ld(LW[:, :, :], lruw.rearrange("a i o -> i a o"))
                for g in range(4):
                    for d in range(2):
                        K.act(smalls[:, 2:3], pcol("lru_lam", d, g), AF.Exp, scale=-1.0)
                        K.act(smalls[:, 2:3], smalls[:, 2:3], AF.Ln, bias=1.0)
                        K.ts(smalls[:, 3:4], smalls[:, 2:3], -8.0, ALU.mult)
                        K.ts(smalls[:, 4:5], smalls[:, 2:3], -16.0, ALU.mult)
                        K.memset(NST[:, :], 0.0)
                        for (lo, hi, rev) in blocks(d):
                            n = hi - lo
                            slo, shi = (0, LC) if lo < LC else (LC, NPOS)
                            a0 = max(lo - 2, slo)
                            a1 = min(hi + 1, shi)
                            xh = MX[0]
                            K.memset(xh[:, 0:n + 3], 0.0)
                            K.ld(View(xh.t[:, a0 - (lo - 2):a1 - (lo - 2)], xh.k), PT[2560 + g * 128:2560 + (g + 1) * 128, a0:a1])
                            xc = MX[1]
                            K.ts(xc[:, 0:n], xh[:, 2:2 + n], pcol("lru_cw", 2, g), ALU.mult, pcol("lru_cb", g), ALU.add)
                            K.stt(xc[:, 0:n], xh[:, 0:n], pcol("lru_cw", 0, g), xc[:, 0:n], ALU.mult, ALU.add)
                            K.stt(xc[:, 0:n], xh[:, 1:1 + n], pcol("lru_cw", 1, g), xc[:, 0:n], ALU.mult, ALU.add)
                            K.stt(xc[:, 0:n], xh[:, 3:3 + n], pcol("lru_cw", 3, g), xc[:, 0:n], ALU.mult, ALU.add)
                            for c in range((n + 511) // 512):
                                cn = min(512, n - c * 512)
                                sl = slice(c * 512, c * 512 + cn)
                                K.mm(pbank[0][:, 0:cn], LW[:, (d * 2 + 0) * 4 + g, :], xc[:, sl])
                                K.mm(pbank[1][:, 0:cn], LW[:, (d * 2 + 1) * 4 + g, :], xc[:, sl])
                                K.act(MX[2][:, sl], pbank[0][:, 0:cn], AF.Sigmoid, bias=pcol("lru_ba", d, g))
                                K.act(MX[3][:, sl], pbank[1][:, 0:cn], AF.Sigmoid, bias=pcol("lru_bi", d, g))
                            K.act(MX[4][:, 0:n], MX[2][:, 0:n], AF.Exp, scale=smalls[:, 3:4])
                            K.act(MX[5][:, 0:n], MX[2][:, 0:n], AF.Exp, scale=smalls[:, 4:5])
                            K.act(MX[5][:, 0:n], MX[5][:, 0:n], AF.Sqrt, scale=-1.0, bias=1.0)
                            K.tt(MX[5][:, 0:n], MX[5][:, 0:n], MX[3][:, 0:n], ALU.mult)
                            K.tt(MX[5][:, 0:n], MX[5][:, 0:n], xc[:, 0:n], ALU.mult)
                            K.scan(rv(MX[6], n, rev), rv(MX[4], n, rev), rv(MX[5], n, rev), NST[:, 0:1])
                            lastc = 0 if rev else n - 1
                            K.cp(NST[:, 0:1], MX[6][:, lastc:lastc + 1], e="act")
                            if d == 0:
                                K.stq(OT[0][g * 128:(g + 1) * 128, lo:hi], MX[6][:, 0:n])
                            elif lo >= LC:
                                K.ld(MX[7][:, 0:n], OT[0][g * 128:(g + 1) * 128, lo:hi])
                                K.tt(MX[6][:, 0:n], MX[6][:, 0:n], MX[7][:, 0:n], ALU.add)
                                gb = MX[8]
                                K.ld(gb[:, 0:n], PT[3072 + g * 128:3072 + (g + 1) * 128, lo:hi])
                                K.tt(MX[7][:, 0:n], gb[:, 0:n], gb[:, 0:n], ALU.mult)
                                K.ts(MX[7][:, 0:n], MX[7][:, 0:n], 0.044715, ALU.mult, 1.0, ALU.add)
                                K.tt(MX[7][:, 0:n], MX[7][:, 0:n], gb[:, 0:n], ALU.mult)
                                K.act(MX[7][:, 0:n], MX[7][:, 0:n], AF.Sigmoid, scale=1.5957691216057308)
                                K.tt(MX[7][:, 0:n], MX[7][:, 0:n], gb[:, 0:n], ALU.mult)
                                K.tt(MXB[0][:, 0:n], MX[6][:, 0:n], MX[7][:, 0:n], ALU.mult)
                                K.stq(YT[512 + g * 128:512 + (g + 1) * 128, lo:hi], MXB[0][:, 0:n])
                        P.barrier()
            P.barrier()

            wob = WA1.t[:, 0:8 * D].rearrange("p (k n) -> p k n", k=8)
            for g in range(2):
                stg = SA.t[:, :].rearrange("p (k n) -> p k n", k=8)
                K.P.dma(stg, outw[layer].rearrange("(k p) n -> p k n", p=128)[:, :, g * 512:(g + 1) * 512], writes=[SA.k])
                K.cp(WA1.v(wob[:, :, g * 512:(g + 1) * 512]), SA.v(stg))
            for (seg, poff, slen) in SEGS:
                if last and seg == 0:
                    continue
                load_mod(G4[1], layer, seg, 4, n2g[layer])
                load_mod(G4[2], layer, seg, 3)
                load_mod(G4[4], layer, seg, 2)
                for t0 in range(0, slen, 128):
                    par = (t0 // 128) % 2
                    ytb = (T2[0], T2X[0])[par]
                    vtb = (T2[1], T2X[1])[par]
                    hb_ = (G4[0], G4[5])[par]
                    ub_ = G2[par]
                    K.ld(ytb[:, :, :], YT.rearrange("(k p) t -> p k t", p=128)[:, :, poff + t0:poff + t0 + 128])
                    K.ld(hb_[:, :], hrows(layer, seg, t0, 128))
                    for half in range(2):
                        pb_ = pbank[2 * par + half]
                        for k in range(8):
                            K.mm(pb_[:, :], ytb[:, k, :], WA1.v(wob[:, k, half * 512:(half + 1) * 512]), start=(k == 0), stop=(k == 7))
                        K.tt(G4[3][:, half * 512:(half + 1) * 512], pb_[:, :], G4[4][:, half * 512:(half + 1) * 512], ALU.mult)
                    K.tt(hb_[:, :], hb_[:, :], G4[3][:, :], ALU.add)
                    K.stq(hrows_out(layer, seg, t0, 128), hb_[:, :])
                    norm_mod(hb_[:, :], G4[1][:, :], G4[2][:, :], ub_[:, :], G4[3][:, :])
                    for k in range(8):
                        K.tr(pst[:, k, :], ub_[:, k * 128:(k + 1) * 128], idb[:, :])
                    K.cp(vtb[:, :, :], pst[:, :, :], e="act")
                    K.stq(VT.rearrange("(k p) t -> p k t", p=128)[:, :, poff + t0:poff + t0 + 128], vtb[:, :, :])
            P.barrier()

            HF = DFF // 2
            for hf in range(2):
                w1b = WA1.t[:, 0:8 * HF].rearrange("p (k n) -> p k n", k=8)
                w3b = WA2.t[:, 0:8 * HF].rearrange("p (k n) -> p k n", k=8)
                for (wsrc, wdst, wbuf) in ((w1, w1b, WA1), (w3, w3b, WA2)):
                    for g0 in range(0, HF, 512):
                        gn = min(512, HF - g0)
                        stg = SA.t[:, :].rearrange("p (k n) -> p k n", k=8)
                        K.P.dma(stg[:, :, 0:gn], wsrc[layer].rearrange("(k p) n -> p k n", p=128)[:, :, hf * HF + g0:hf * HF + g0 + gn], writes=[SA.k])
                        K.cp(wbuf.v(wdst[:, :, g0:g0 + gn]), SA.v(stg[:, :, 0:gn]))
                ci = 0
                for (seg, poff, slen) in SEGS:
                    if last and seg == 0:
                        continue
                    bs = min(512, slen)
                    for b0 in range(0, slen, bs):
                        K.ld(T8[:, :, 0:bs], VT.rearrange("(k p) t -> p k t", p=128)[:, :, poff + b0:poff + b0 + bs])
                        for fc in range(HF // 128):
                            p1 = pbank[(2 * fc) % 6]
                            p3 = pbank[(2 * fc + 1) % 6]
                            for k in range(8):
                                K.mm(p1[:, 0:bs], WA1.v(w1b[:, k, fc * 128:(fc + 1) * 128]), T8[:, k, 0:bs], start=(k == 0), stop=(k == 7))
                            for k in range(8):
                                K.mm(p3[:, 0:bs], WA2.v(w3b[:, k, fc * 128:(fc + 1) * 128]), T8[:, k, 0:bs], start=(k == 0), stop=(k == 7))
                            sl_ = STG[ci % 3]
                            K.act(sl_[:, 0:bs], p1[:, 0:bs], AF.Silu)
                            sg = ASTG[ci % 3]
                            ci += 1
                            K.tt(sg[:, 0:bs], sl_[:, 0:bs], p3[:, 0:bs], ALU.mult)
                            r0 = hf * HF + fc * 128
                            K.stq(AT[r0:r0 + 128, poff + b0:poff + b0 + bs], sg[:, 0:bs])
                P.barrier()

            def w2v(k, c0, c1):
                if k < 16:
                    return View(WA1.t[:, :].rearrange("p (k n) -> p k n", k=16)[:, k, c0:c1], WA1.k)
                return View(WA2.t[:, :].rearrange("p (k n) -> p k n", k=16)[:, k - 16, c0:c1], WA2.k)
            for kc in range(22):
                for g in range(1):
                    K.P.dma(SA.t[:, 0:D], w2[layer].rearrange("(k p) n -> p k n", p=128)[:, kc, :], writes=[SA.k])
                    K.cp(w2v(kc, 0, D), SA[:, 0:D])
            if last:
                K.ld(G4[2][:, :], fing.partition_broadcast(128))
            for (seg, poff, slen) in SEGS:
                if last and seg == 0:
                    continue
                load_mod(G4[4], layer, seg, 5)
                for t0 in range(0, slen, 128):
                    par = (t0 // 128) % 2
                    atb = (ATt, ATt2)[par]
                    hb_ = (G4[0], G4[5])[par]
                    tb_ = (G4[3], G4[1])[par]
                    K.ld(atb[:, :, :], AT.rearrange("(k p) t -> p k t", p=128)[:, :, poff + t0:poff + t0 + 128])
                    K.ld(hb_[:, :], hrows_out(layer, seg, t0, 128))
                    for half in range(2):
                        pb_ = pbank[2 * par + half]
                        for k in range(22):
                            K.mm(pb_[:, :], atb[:, k, :], w2v(k, half * 512, (half + 1) * 512), start=(k == 0), stop=(k == 21))
                        K.tt(tb_[:, half * 512:(half + 1) * 512], pb_[:, :], G4[4][:, half * 512:(half + 1) * 512], ALU.mult)
                    K.tt(hb_[:, :], hb_[:, :], tb_[:, :], ALU.add)
                    if last:
                        K.act(tb_[:, :], hb_[:, :], AF.Square, accum=ssb[:, :])
                        K.act(ssb[:, :], ssb[:, :], AF.Sqrt, scale=1.0 / D, bias=EPS)
                        K.recip(ssb[:, :], ssb[:, :])
                        K.stt(tb_[:, :], hb_[:, :], ssb[:, 0:1], G4[2][:, :], ALU.mult, ALU.mult)
                        K.stq(hrows_out(layer, seg, t0, 128, final=True), tb_[:, :])
                    else:
                        K.stq(hrows_out(layer, seg, t0, 128), hb_[:, :])
            P.barrier()
        print("instructions:", P.ninstr)
    return nc


def prep_inputs(inp):
    f = np.float32
    g = lambda k: np.asarray(inp[k], dtype=f)
    ev = g("ev_in_w")[0]
    splits = np.cumsum([0, 512, 512, 512, 128, 128, 128, 512, 512, 512, 512, 8, 8])
    inw0 = np.zeros((D, 4096), f)
    inw0[:, :3984] = ev
    pvv = np.zeros((128, NPV), f)

    def setc(name, vec):
        pvv[:, PVL[name]] = vec
    cw = g("lru_conv_w")[0]
    for gi in range(4):
        sl = slice(gi * 128, (gi + 1) * 128)
        for j in range(4):
            setc(("lru_cw", j, gi), cw[j, sl])
        setc(("lru_cb", gi), g("lru_conv_b")[0, sl])
        for d in range(2):
            setc(("lru_ba", d, gi), g("lru_ba")[0, d, sl])
            setc(("lru_bi", d, gi), g("lru_bi")[0, d, sl])
            setc(("lru_lam", d, gi), g("lru_lam")[0, d, sl])
    for h in range(4):
        sl = slice(h * 128, (h + 1) * 128)
        setc(("hg_lb0", h), g("hgrn_lb")[0, sl])
        setc(("hg_lb1", h), g("hgrn_lb")[1, sl])
        setc(("hg_ng", h), g("hgrn_ng")[0, sl])
        setc(("ml_ng", h), g("mlstm_ng")[0, sl])
        for d in range(2):
            setc(("ml_bi", d, h), np.full(128, g("mlstm_bi")[0, d, h], f))
            setc(("ml_bf", d, h), np.full(128, g("mlstm_bf")[0, d, h], f))
    mu = g("rwkv_mu")[0]
    for d in range(2):
        for c in range(4):
            sl = slice(c * 128, (c + 1) * 128)
            setc(("mu_r", d, c), mu[d, 0:512][sl])
            setc(("mu_k", d, c), mu[d, 512:1024][sl])
            setc(("mu_v", d, c), mu[d, 1024:1536][sl])
            setc(("w0", d, c), g("rwkv_w0")[0, d, sl])
            setc(("a0", d, c), g("rwkv_a0")[0, d, sl])
            setc(("k_k", d, c), g("rwkv_kk")[0, d, sl])
            setc(("k_a", d, c), g("rwkv_ka")[0, d, sl])
            setc(("r_k", d, c), g("rwkv_rk")[0, d].reshape(-1)[sl])
            setc(("ln_w", d, c), g("rwkv_lnw")[0, d, sl])
            setc(("ln_b", d, c), g("rwkv_lnb")[0, d, sl])
        pvv[0:64, PVL[("mu_wd", d)]] = mu[d, 1536:1600]
        pvv[0:64, PVL[("mu_ad", d)]] = mu[d, 1600:1664]
    bones_ = np.zeros((128, 128), f)
    bones_[0:64, 0:64] = 1.0
    bones_[64:128, 64:128] = 1.0
    ii = np.arange(128)
    sgrid, tgrid = np.meshgrid(ii, ii, indexing="ij")
    same = (sgrid // 64) == (tgrid // 64)
    masks_ = np.stack([sgrid <= tgrid, sgrid >= tgrid, same & (sgrid <= tgrid), same & (sgrid >= tgrid),
                       same & (sgrid < tgrid), same & (sgrid > tgrid)]).astype(f)
    lruw = np.zeros((2, 2, 4, 128, 128), f)
    for d in range(2):
        for wi_, nm in enumerate(("lru_wa", "lru_wi")):
            w = g(nm)[0, d]
            for gi in range(4):
                for bb in range(2):
                    lruw[d, wi_, gi, bb * 64:(bb + 1) * 64, bb * 64:(bb + 1) * 64] = w[gi * 2 + bb]
    shared = {
        "n1g": g("norm1_g"), "n2g": g("norm2_g"), "fing": g("final_g"),
        "modw": g("mod_w"), "modb": g("mod_b"), "w1": g("ffn_w1"), "w3": g("ffn_w3"), "w2": g("ffn_w2"),
        "inw0": inw0, "inw1": np.ascontiguousarray(g("od_in_w")[0]),
        "outw0": np.ascontiguousarray(g("ev_out_w")[0]), "outw1": np.ascontiguousarray(g("od_out_w")[0]),
        "pv": pvv, "identd": np.eye(128, dtype=f), "eyeflat": np.eye(128, dtype=f).reshape(-1),
        "lruw": lruw.reshape(16, 128, 128),
        "rw2": np.ascontiguousarray(g("rwkv_w2")[0]), "ra2": np.ascontiguousarray(g("rwkv_a2")[0]),
        "rg2": np.ascontiguousarray(g("rwkv_g2")[0]), "bonesd": bones_, "masksd": masks_,
    }
    maps = []
    for core in range(8):
        b = core % 4
        m = dict(shared)
        m["x"] = np.ascontiguousarray(g("x")[b])
        m["ctx"] = np.ascontiguousarray(g("ctx")[b])
        m["cvec"] = np.stack([g("c")[b], g("c_ctx")])
        maps.append(m)
    return maps


_NC = None


def kernel(**inputs):
    global _NC
    if _NC is None:
        _NC = build()
    maps = prep_inputs(inputs)
    res = run_bass_kernel_spmd(_NC, maps, core_ids=list(range(8)))
    return np.stack([res.results[b]["out"] for b in range(4)]).astype(np.float32)
```
